# Optimizing a Trainium2 kernel written in Bass

```python
import jax, jax.numpy as jnp
from jax import lax
import numpy as np

D_MODEL = 1024
BATCH = 8
SEQ = 4096
DEPTH = 4

CHUNK = 64
HEAD_DIM = 64
N_HEADS_A = 8
N_HEADS_B = 8
WIDTH_A = N_HEADS_A * HEAD_DIM
WIDTH_B = N_HEADS_B * HEAD_DIM
LEFT_CHUNKS = 8
BAND = (LEFT_CHUNKS + 1) * CHUNK
MAX_REL = 128
N_REL = 2 * MAX_REL + 1
SB_BLOCK = 128
N_EXPERTS = 16
N_GROUPS = 4
EXPERTS_PER_GROUP = N_EXPERTS // N_GROUPS
TOP_K = 2
D_EXPERT = 512
ALPHA = (2 * DEPTH) ** 0.25
BETA_INIT = (8 * DEPTH) ** -0.25
LN_EPS = 1e-5
SPLIT_A = 3 * WIDTH_A
SPLIT_B = SPLIT_A + 3 * WIDTH_B
PROJ_COLS = SPLIT_B + 2 * D_MODEL

kernel_name = "hybrid_chunked_relpos_stickbreaking_grouped_moe_deepnorm"


def layer_norm(x, g, b):
    xf = x.astype(jnp.float32)
    mu = jnp.mean(xf, axis=-1, keepdims=True)
    var = jnp.mean(jnp.square(xf - mu), axis=-1, keepdims=True)
    return ((xf - mu) * lax.rsqrt(var + LN_EPS)).astype(x.dtype) * g + b


def chunked_relpos_attention(q, k, v, rel_bias):
    b, h, s, dh = q.shape
    n_chunks = s // CHUNK
    pad = LEFT_CHUNKS * CHUNK
    k_pad = jnp.pad(k, ((0, 0), (0, 0), (pad, 0), (0, 0)))
    v_pad = jnp.pad(v, ((0, 0), (0, 0), (pad, 0), (0, 0)))
    q_chunks = q.reshape(b, h, n_chunks, CHUNK, dh).transpose(2, 0, 1, 3, 4)
    qi = jnp.arange(CHUNK)[:, None]
    kj = jnp.arange(BAND)[None, :]
    dist = pad + qi - kj
    rel_idx = jnp.clip(dist, -MAX_REL, MAX_REL) + MAX_REL
    bias = rel_bias[:, rel_idx].astype(jnp.float32)
    scale = dh ** -0.5

    def one_chunk(args):
        c, qc = args
        start = c * CHUNK
        kb = lax.dynamic_slice_in_dim(k_pad, start, BAND, axis=2)
        vb = lax.dynamic_slice_in_dim(v_pad, start, BAND, axis=2)
        scores = jnp.einsum('bhqd,bhkd->bhqk', qc, kb).astype(jnp.float32) * scale + bias
        valid = (kj + start - pad) >= 0
        scores = jnp.where(valid[None, None], scores, -jnp.inf)
        p = jax.nn.softmax(scores, axis=-1).astype(v.dtype)
        return jnp.einsum('bhqk,bhkd->bhqd', p, vb)

    out = lax.map(one_chunk, (jnp.arange(n_chunks), q_chunks))
    return out.transpose(1, 2, 0, 3, 4).reshape(b, h, s, dh)


def stick_breaking_attention(q, k, v):
    b, h, s, dh = q.shape
    n_blocks = s // SB_BLOCK
    scale = dh ** -0.5
    q_blocks = q.reshape(b, h, n_blocks, SB_BLOCK, dh).transpose(2, 0, 1, 3, 4)
    key_pos = jnp.arange(s)

    def one_block(args):
        blk, qb = args
        q_pos = blk * SB_BLOCK + jnp.arange(SB_BLOCK)
        z = jnp.einsum('bhqd,bhkd->bhqk', qb, k).astype(jnp.float32) * scale
        before = (key_pos[None, :] < q_pos[:, None])[None, None]
        log_beta = jax.nn.log_sigmoid(z)
        log_keep = jnp.where(before, jax.nn.log_sigmoid(-z), 0.0)
        suffix = lax.cumsum(log_keep, axis=3, reverse=True) - log_keep
        a = jnp.where(before, jnp.exp(log_beta + suffix), 0.0).astype(v.dtype)
        return jnp.einsum('bhqk,bhkd->bhqd', a, v)

    out = lax.map(one_block, (jnp.arange(n_blocks), q_blocks))
    return out.transpose(1, 2, 0, 3, 4).reshape(b, h, s, dh)


def hybrid_mixer(x, w_in, rel_bias, w_up_a, w_up_b, w_out):
    b, s, _ = x.shape
    proj = x @ w_in
    qkv_a, qkv_b, gate_logits = jnp.split(proj, [SPLIT_A, SPLIT_B], axis=-1)

    def to_heads(t, n):
        return t.reshape(b, s, 3, n, HEAD_DIM).transpose(2, 0, 3, 1, 4)

    qa, ka, va = to_heads(qkv_a, N_HEADS_A)
    qb, kb, vb = to_heads(qkv_b, N_HEADS_B)
    ya = chunked_relpos_attention(qa, ka, va, rel_bias)
    yb = stick_breaking_attention(qb, kb, vb)
    ya = ya.transpose(0, 2, 1, 3).reshape(b, s, WIDTH_A) @ w_up_a
    yb = yb.transpose(0, 2, 1, 3).reshape(b, s, WIDTH_B) @ w_up_b
    gate_a, gate_b = jnp.split(jax.nn.sigmoid(gate_logits), 2, axis=-1)
    return (gate_a * ya + gate_b * yb) @ w_out


def grouped_top2_moe(x, w_router, b_router, w_gate, w_up, w_down):
    b, s, d = x.shape
    x2d = x.reshape(b * s, d)
    logits = x2d.astype(jnp.float32) @ w_router.astype(jnp.float32) + b_router.astype(jnp.float32)
    probs = jax.nn.softmax(logits, axis=-1)
    grouped = probs.reshape(-1, N_GROUPS, EXPERTS_PER_GROUP)
    group_idx = jnp.argmax(jnp.max(grouped, axis=-1), axis=-1)
    in_group = jnp.take_along_axis(grouped, group_idx[:, None, None], axis=1)[:, 0]
    top_w, top_i = lax.top_k(in_group, TOP_K)
    top_w = top_w / jnp.sum(top_w, axis=-1, keepdims=True)
    expert_id = group_idx[:, None] * EXPERTS_PER_GROUP + top_i
    combine = jnp.sum(jax.nn.one_hot(expert_id, N_EXPERTS, dtype=jnp.float32) * top_w[..., None], axis=1)
    combine = combine.astype(x.dtype)
    out = jnp.zeros_like(x2d)
    for e in range(N_EXPERTS):
        hdn = jax.nn.silu(x2d @ w_gate[e]) * (x2d @ w_up[e])
        out = out + combine[:, e:e + 1] * (hdn @ w_down[e])
    return out.reshape(b, s, d)


def setup_inputs(seed: int = 0) -> dict:
    key = jax.random.key(seed)
    ks = jax.random.split(key, 16)
    f32 = jnp.float32
    col_scale = np.ones((PROJ_COLS,), np.float32)
    col_scale[2 * WIDTH_A:3 * WIDTH_A] = BETA_INIT
    col_scale[SPLIT_A + 2 * WIDTH_B:SPLIT_B] = BETA_INIT
    x = jax.random.normal(ks[0], (BATCH, SEQ, D_MODEL), f32)
    w_in = jax.random.normal(ks[1], (DEPTH, D_MODEL, PROJ_COLS), f32) * D_MODEL ** -0.5 * jnp.asarray(col_scale)
    rel_bias = jax.random.normal(ks[2], (DEPTH, N_HEADS_A, N_REL), f32) * 0.1
    w_up_a = jax.random.normal(ks[3], (DEPTH, WIDTH_A, D_MODEL), f32) * WIDTH_A ** -0.5 * BETA_INIT
    w_up_b = jax.random.normal(ks[4], (DEPTH, WIDTH_B, D_MODEL), f32) * WIDTH_B ** -0.5 * BETA_INIT
    w_out = jax.random.normal(ks[5], (DEPTH, D_MODEL, D_MODEL), f32) * D_MODEL ** -0.5 * BETA_INIT
    ln1_g = 1.0 + 0.02 * jax.random.normal(ks[6], (DEPTH, D_MODEL), f32)
    ln1_b = 0.02 * jax.random.normal(ks[7], (DEPTH, D_MODEL), f32)
    w_router = jax.random.normal(ks[8], (D_MODEL, N_EXPERTS), f32) * D_MODEL ** -0.5
    b_router = 0.01 * jax.random.normal(ks[9], (N_EXPERTS,), f32)
    w_gate = jax.random.normal(ks[10], (DEPTH, N_EXPERTS, D_MODEL, D_EXPERT), f32) * D_MODEL ** -0.5 * BETA_INIT
    w_up = jax.random.normal(ks[11], (DEPTH, N_EXPERTS, D_MODEL, D_EXPERT), f32) * D_MODEL ** -0.5 * BETA_INIT
    w_down = jax.random.normal(ks[12], (DEPTH, N_EXPERTS, D_EXPERT, D_MODEL), f32) * D_EXPERT ** -0.5 * BETA_INIT
    ln2_g = 1.0 + 0.02 * jax.random.normal(ks[13], (DEPTH, D_MODEL), f32)
    ln2_b = 0.02 * jax.random.normal(ks[14], (DEPTH, D_MODEL), f32)
    return {"x": x, "w_in": w_in, "rel_bias": rel_bias, "w_up_a": w_up_a, "w_up_b": w_up_b,
            "w_out": w_out, "ln1_g": ln1_g, "ln1_b": ln1_b, "w_router": w_router,
            "b_router": b_router, "w_gate": w_gate, "w_up": w_up, "w_down": w_down,
            "ln2_g": ln2_g, "ln2_b": ln2_b}


def reference(x, w_in, rel_bias, w_up_a, w_up_b, w_out, ln1_g, ln1_b, w_router,
              b_router, w_gate, w_up, w_down, ln2_g, ln2_b):
    for l in range(DEPTH):
        mixed = hybrid_mixer(x, w_in[l], rel_bias[l], w_up_a[l], w_up_b[l], w_out[l])
        x = layer_norm(ALPHA * x + mixed, ln1_g[l], ln1_b[l])
        ffn = grouped_top2_moe(x, w_router, b_router, w_gate[l], w_up[l], w_down[l])
        x = layer_norm(ALPHA * x + ffn, ln2_g[l], ln2_b[l])
    return x
```

```python
import contextlib
import numpy as np
import concourse.bass as bass
import concourse.mybir as mybir
from concourse.bass_utils import run_bass_kernel_spmd

F32 = mybir.dt.float32
BF16 = mybir.dt.bfloat16
I32 = mybir.dt.int32
AF = mybir.ActivationFunctionType
ALU = mybir.AluOpType
AX = mybir.AxisListType

DEPTH = 4
D = 1024
S_LEN = 4096
NG = 8
GT = 512
NE = 16
DF = 512
ALPHA = (2 * DEPTH) ** 0.25
LN_EPS = 1e-5
NEG = -30000.0

ENGS = ["pe", "act", "dve", "pool", "sp"]
NSEM = {"sp": 40, "pool": 24, "act": 8}


class Buf:
    __slots__ = ("ap", "name", "lw", "rd")

    def __init__(self, ap, name=""):
        self.ap = ap
        self.name = name
        self.lw = None
        self.rd = []

    def __getitem__(self, idx):
        return self.ap[idx]


class Sched:
    def __init__(self, nc, stack):
        self.nc = nc
        self.streams = {e: [] for e in ENGS}
        self.count = {e: 0 for e in ENGS}
        self.sem = {e: stack.enter_context(nc.semaphore("prog_" + e)) for e in ENGS}
        self.dsem = {q: [stack.enter_context(nc.semaphore("dma_%s_%d" % (q, i))) for i in range(n)]
                     for q, n in NSEM.items()}
        self.dcount = {q: 0 for q in NSEM}
        self.seen = {e: {} for e in ENGS}
        self.pending = {e: [] for e in ENGS}

    def _deps(self, reads, writes):
        deps = []
        for b in reads:
            if b.lw is not None:
                deps.append(b.lw)
        for b in writes:
            if b.lw is not None:
                deps.append(b.lw)
            deps.extend(b.rd)
        return deps

    def _mark(self, tok, reads, writes):
        for b in reads:
            b.rd.append(tok)
            if len(b.rd) > 48:
                b.rd = b.rd[-48:]
        for b in writes:
            b.lw = tok
            b.rd = []

    def _waits(self, eng, deps, skip_same_pe=False):
        if self.pending[eng]:
            deps = list(deps) + self.pending[eng]
            self.pending[eng] = []
        need = {}
        for (kind, a, b) in deps:
            if kind == "c":
                if skip_same_pe and a == "pe" and eng == "pe":
                    continue
                key = ("c", a)
            else:
                key = ("d", a)
            if self.seen[eng].get(key, 0) >= b:
                continue
            if need.get(key, 0) < b:
                need[key] = b
        out = []
        for key, val in need.items():
            self.seen[eng][key] = val
            if key[0] == "c":
                sem = self.sem[key[1]]
            else:
                sem = self.dsem[key[1][0]][key[1][1]]
            out.append((sem, val))
        return out

    def op(self, eng, fn, reads=(), writes=(), pe_acc=False):
        deps = self._deps(reads, writes)
        waits = self._waits(eng, deps, skip_same_pe=pe_acc)
        self.count[eng] += 1
        tok = ("c", eng, self.count[eng])
        self.streams[eng].append((_freeze(fn), waits, (self.sem[eng], 1)))
        self._mark(tok, reads, writes)
        return tok

    def dma(self, q, fn, reads=(), writes=()):
        deps = self._deps(reads, writes)
        n = NSEM[q]
        slot = self.dcount[q] % n
        gen = self.dcount[q] // n
        self.dcount[q] += 1
        if gen > 0:
            deps.append(("d", (q, slot), 16 * gen))
        waits = self._waits(q, deps)
        tok = ("d", (q, slot), 16 * (gen + 1))
        self.streams[q].append((_freeze(fn), waits, (self.dsem[q][slot], 16)))
        self._mark(tok, reads, writes)
        return tok

    def all_tokens(self):
        toks = [("c", e, self.count[e]) for e in ENGS if self.count[e] > 0]
        for q, n in NSEM.items():
            for slot in range(min(n, self.dcount[q])):
                last = ((self.dcount[q] - 1 - slot) // n)
                toks.append(("d", (q, slot), 16 * (last + 1)))
        return toks

    def barrier(self, dummy):
        toks = self.all_tokens()
        self.pending["pool"].extend(toks)
        tb = self.op("pool", lambda e: e.memset(dummy.ap[:], 0.0), writes=[dummy])
        for e in ENGS:
            if e != "pool":
                self.pending[e].append(tb)

    def finish(self, eng="sp"):
        waits = self._waits(eng, self.all_tokens())
        self.streams[eng].append((None, waits, None))

    def emit(self):
        nc = self.nc
        streams = self.streams
        with nc.Block() as block:
            def run(engine, items):
                for fn, waits, inc in items:
                    for sem, val in waits:
                        engine.wait_ge(sem, val)
                    if fn is not None:
                        name, a, k = fn
                        getattr(engine, name)(*a, **k).then_inc(inc[0], inc[1])

            @block.tensor
            def _(e):
                run(e, streams["pe"])

            @block.scalar
            def _(e):
                run(e, streams["act"])

            @block.vector
            def _(e):
                run(e, streams["dve"])

            @block.gpsimd
            def _(e):
                run(e, streams["pool"])

            @block.sync
            def _(e):
                run(e, streams["sp"])


class _Rec:
    def __init__(self):
        self.call = None

    def __getattr__(self, name):
        def f(*a, **k):
            self.call = (name, a, k)
            return self
        return f


def _freeze(fn):
    r = _Rec()
    fn(r)
    assert r.call is not None
    return r.call


class Rot:
    def __init__(self, bufs):
        self.bufs = bufs
        self.i = 0

    def next(self):
        b = self.bufs[self.i % len(self.bufs)]
        self.i += 1
        return b


def build_nc(n_layers=DEPTH, stop_after=None, debug=False):
    nc = bass.Bass("TRN2", target_bir_lowering=False)
    dt_in = lambda name, shape: nc.dram_tensor(name, list(shape), F32, kind="ExternalInput").ap()
    xT_in = dt_in("xT", [D, S_LEN])
    w_in = dt_in("w_in", [DEPTH, D, 5120])
    relB = dt_in("relB", [DEPTH, 8, 128, 640])
    w_up_a = dt_in("w_up_a", [DEPTH, 512, D])
    w_up_b = dt_in("w_up_b", [DEPTH, 512, D])
    w_out = dt_in("w_out", [DEPTH, D, D])
    lnp = dt_in("lnp", [128, DEPTH * 4 * 8])
    w_router = dt_in("w_router", [D, NE])
    brb = dt_in("brb", [128, NE])
    w_gate = dt_in("w_gate", [DEPTH, NE, D, DF])
    w_up = dt_in("w_up", [DEPTH, NE, D, DF])
    w_down = dt_in("w_down", [DEPTH, NE, DF, D])
    yT_out = nc.dram_tensor("yT", [D, S_LEN], F32, kind="ExternalOutput").ap()

    def scratch(name, shape, dt):
        kind = "ExternalOutput" if (debug and name in debug) else "Internal"
        return nc.dram_tensor(name, list(shape), dt, kind=kind).ap()

    wb_in = [scratch("wb_in%d" % l, [D, 5120], BF16) for l in range(n_layers)]
    wb_upa = [scratch("wb_upa%d" % l, [512, D], BF16) for l in range(n_layers)]
    wb_upb = [scratch("wb_upb%d" % l, [512, D], BF16) for l in range(n_layers)]
    wb_out = [scratch("wb_out%d" % l, [D, D], BF16) for l in range(n_layers)]
    wb_g = [scratch("wb_g%d" % l, [NE * 128, 8 * DF], BF16) for l in range(n_layers)]
    wb_u = [scratch("wb_u%d" % l, [NE * 128, 8 * DF], BF16) for l in range(n_layers)]
    wb_d = [scratch("wb_d%d" % l, [NE * 128, 4 * D], BF16) for l in range(n_layers)]
    TS = 256
    SUB = TS // 128
    NTL = 32 + NE
    NSLOT = NTL * TS
    x1tok = scratch("x1tok", [S_LEN, D], BF16)
    slotmap = scratch("slotmap", [NSLOT, 2], F32)
    oslot = scratch("oslot", [NSLOT, D], F32)
    xres = scratch("xres", [8, 128, S_LEN], F32)
    qkT = scratch("qkT", [16, 128, S_LEN], BF16)
    Vd = scratch("Vd", [S_LEN, 1024], BF16)
    YTd = scratch("YTd", [8, 128, S_LEN], BF16)
    gTd = scratch("gTd", [16, 128, S_LEN], BF16)
    dbg = {}
    if debug:
        for nm in debug:
            pass

    with contextlib.ExitStack() as st:
        S = Sched(nc, st)

        uid = [0]

        def sb(stack, name, shape, dt):
            uid[0] += 1
            return stack.enter_context(nc.sbuf_tensor("%s_%d" % (name, uid[0]), list(shape), dt))

        def ps(stack, name, shape, dt=F32):
            uid[0] += 1
            return stack.enter_context(nc.psum_tensor("%s_%d" % (name, uid[0]), list(shape), dt))

        xbf_t = sb(st, "xbf", [128, 8, S_LEN], BF16)
        xbf = [Buf(xbf_t[:, :, g * GT:(g + 1) * GT], "xbf%d" % g) for g in range(NG)]
        dummy = Buf(sb(st, "bar_dummy", [128, 8], F32))
        lnp_sb = Buf(sb(st, "lnp_sb", [128, DEPTH * 4 * 8], F32))
        ones_d = Buf(sb(st, "ones_d", [128, 128], F32))
        ident = Buf(sb(st, "ident", [128, 128], F32))
        identb = Buf(sb(st, "identb", [128, 128], BF16))
        trimask = Buf(sb(st, "trimask", [128, 128], BF16))
        ntri = Buf(sb(st, "ntri", [128, 128], BF16))
        nstr = Buf(sb(st, "nstr", [128, 128], BF16))
        wr32 = Buf(sb(st, "wr32", [128, 8, NE], F32))
        brb_sb = Buf(sb(st, "brb_sb", [128, NE], F32))
        pstr = Buf(sb(st, "pstr", [128, 128], BF16))
        pones = Buf(sb(st, "pones", [128, 128], BF16))
        pidx = Buf(sb(st, "pidx", [128, 1], F32))
        tokid = Buf(sb(st, "tokid", [128, 32], F32))
        tvals = Buf(sb(st, "tvals", [128, 48, NE], F32))
        zeros = Buf(sb(st, "zeros", [128, 256], F32))
        maskM = Buf(sb(st, "maskM", [128, 640], F32))
        lg_glob = Buf(sb(st, "lg_glob", [128, 32, NE], F32))
        itmp = Buf(sb(st, "itmp", [128, 48 * NE], I32))

        S.dma("sp", lambda e: e.dma_start(out=lnp_sb.ap[:], in_=lnp[:, :]), writes=[lnp_sb])
        S.dma("sp", lambda e: e.dma_start(out=brb_sb.ap[:], in_=brb[:, :]), writes=[brb_sb])
        S.dma("sp", lambda e: e.dma_start(out=wr32.ap[:], in_=w_router.rearrange("(c p) n -> p c n", p=128)), writes=[wr32])
        S.op("pool", lambda e: e.memset(ones_d.ap[:], 1.0 / D), writes=[ones_d])
        for t_, dt_ in ((ident, F32), (identb, BF16)):
            S.op("pool", lambda e, t_=t_: e.memset(t_.ap[:], 0.0), writes=[t_])
            S.op("pool", lambda e, t_=t_: e.affine_select(out=t_.ap[:], in_=t_.ap[:], pattern=[[-1, 128]], compare_op=ALU.not_equal,
                                                           fill=1.0, base=0, channel_multiplier=1), reads=[t_], writes=[t_])
        S.op("pool", lambda e: e.memset(trimask.ap[:], 1.0), writes=[trimask])
        S.op("pool", lambda e: e.affine_select(out=trimask.ap[:], in_=trimask.ap[:], pattern=[[1, 128]], compare_op=ALU.is_gt,
                                               fill=0.0, base=0, channel_multiplier=-1), reads=[trimask], writes=[trimask])
        S.op("pool", lambda e: e.memset(ntri.ap[:], -1.0), writes=[ntri])
        S.op("pool", lambda e: e.affine_select(out=ntri.ap[:], in_=ntri.ap[:], pattern=[[-1, 128]], compare_op=ALU.is_ge,
                                               fill=0.0, base=0, channel_multiplier=1), reads=[ntri], writes=[ntri])
        S.op("pool", lambda e: e.memset(nstr.ap[:], -1.0), writes=[nstr])
        S.op("pool", lambda e: e.affine_select(out=nstr.ap[:], in_=nstr.ap[:], pattern=[[1, 128]], compare_op=ALU.is_gt,
                                               fill=0.0, base=0, channel_multiplier=-1), reads=[nstr], writes=[nstr])
        S.op("pool", lambda e: e.memset(maskM.ap[:], 0.0), writes=[maskM])
        S.op("pool", lambda e: e.affine_select(out=maskM.ap[0:64, :], in_=maskM.ap[0:64, :], pattern=[[-1, 640]], compare_op=ALU.is_ge,
                                               fill=NEG, base=575, channel_multiplier=0), reads=[maskM], writes=[maskM])
        S.op("pool", lambda e: e.affine_select(out=maskM.ap[64:128, :], in_=maskM.ap[64:128, :], pattern=[[1, 640]], compare_op=ALU.is_ge,
                                               fill=NEG, base=-64, channel_multiplier=0), reads=[maskM], writes=[maskM])
        S.op("pool", lambda e: e.memset(pstr.ap[:], 1.0), writes=[pstr])
        S.op("pool", lambda e: e.affine_select(out=pstr.ap[:], in_=pstr.ap[:], pattern=[[1, 128]], compare_op=ALU.is_gt,
                                               fill=0.0, base=0, channel_multiplier=-1), reads=[pstr], writes=[pstr])
        S.op("pool", lambda e: e.memset(pones.ap[:], 1.0), writes=[pones])
        S.op("pool", lambda e: e.memset(zeros.ap[:], 0.0), writes=[zeros])
        S.op("pool", lambda e: e.iota(itmp.ap[:, 0:1], pattern=[[0, 1]], base=0, channel_multiplier=1), writes=[itmp])
        S.op("dve", lambda e: e.tensor_copy(out=pidx.ap[:], in_=itmp.ap[:, 0:1]), reads=[itmp], writes=[pidx])
        S.op("pool", lambda e: e.iota(itmp.ap[:, 0:32], pattern=[[128, 32]], base=0, channel_multiplier=1), reads=[pidx], writes=[itmp])
        S.op("dve", lambda e: e.tensor_copy(out=tokid.ap[:], in_=itmp.ap[:, 0:32]), reads=[itmp], writes=[tokid])
        S.op("pool", lambda e: e.iota(itmp.ap[:], pattern=[[1, 48], [0, NE]], base=0, channel_multiplier=0), reads=[tokid], writes=[itmp])
        S.op("dve", lambda e: e.tensor_copy(out=tvals.ap[:], in_=itmp.ap[:].rearrange("p (t e) -> p t e", e=NE)), reads=[itmp], writes=[tvals])

        def flat2(ap, n):
            names = " ".join("d%d" % i for i in range(ap.ndim))
            flat = ap.rearrange("%s -> (%s)" % (names, names))
            return flat.rearrange("(r c) -> r c", c=1024)

        def cast_weights(l, which):
            items = []
            if which == "mix":
                items = [(wb_in[l], w_in[l]), (wb_upa[l], w_up_a[l]), (wb_upb[l], w_up_b[l]), (wb_out[l], w_out[l])]
            else:
                for e_ in range(NE):
                    for dst, src, a in ((wb_g[l], w_gate[l], 8), (wb_u[l], w_up[l], 8), (wb_d[l], w_down[l], 4)):
                        S.dma("pool", lambda e, dst=dst, src=src, a=a, e_=e_: e.dma_start(
                            out=dst[e_ * 128:(e_ + 1) * 128, :].rearrange("p (c n) -> p c n", c=a),
                            in_=src[e_].rearrange("(c p) n -> p c n", p=128)))
                return
            for dst, src in items:
                n = 1
                for s_ in dst.shape:
                    n *= s_
                d2 = flat2(dst, n)
                s2 = flat2(src, n)
                R = n // 1024
                step = 1024
                for r0 in range(0, R, step):
                    r1 = min(R, r0 + step)
                    S.dma("pool", lambda e, d2=d2, s2=s2, r0=r0, r1=r1: e.dma_start(out=d2[r0:r1, :], in_=s2[r0:r1, :]))

        def cast_gen(l):
            for dst, src in [(wb_in[l], w_in[l]), (wb_upa[l], w_up_a[l]), (wb_upb[l], w_up_b[l]), (wb_out[l], w_out[l])]:
                n = 1
                for s_ in dst.shape:
                    n *= s_
                d2 = flat2(dst, n)
                s2 = flat2(src, n)
                R = n // 1024
                for r0 in range(0, R, 1024):
                    r1 = min(R, r0 + 1024)
                    S.dma("pool", lambda e, d2=d2, s2=s2, r0=r0, r1=r1: e.dma_start(out=d2[r0:r1, :], in_=s2[r0:r1, :]))
                    yield
            for e_ in range(NE):
                for dst, src, a in ((wb_g[l], w_gate[l], 8), (wb_u[l], w_up[l], 8), (wb_d[l], w_down[l], 4)):
                    S.dma("pool", lambda e, dst=dst, src=src, a=a, e_=e_: e.dma_start(
                        out=dst[e_ * 128:(e_ + 1) * 128, :].rearrange("p (c n) -> p c n", c=a),
                        in_=src[e_].rearrange("(c p) n -> p c n", p=128)))
                    yield

        cast_weights(0, "mix")
        for g in range(NG):
            S.dma("pool", lambda e, g=g: e.dma_start(out=xbf[g].ap, in_=xT_in[:, g * GT:(g + 1) * GT].rearrange("(c p) n -> p c n", p=128)),
                  writes=[xbf[g]])

        def lncol(l, which, c):
            i = (l * 4 + which) * 8 + c
            return lnp_sb.ap[:, i:i + 1]

        def layer_norm(ph, l, which, g, y, tbuf, obuf, psA, psB, tmp512, rstd, final, part=0):
            if part in (0, 1):
                ln_part1(y, tbuf, obuf, psA)
            if part in (0, 2):
                ln_part2(l, which, g, tbuf, obuf, psB, tmp512, rstd, final)

        def ln_part1(y, tbuf, obuf, psA):
            for c in range(8):
                S.op("pe", lambda e, c=c: e.matmul(psA.ap[:], ones_d.ap[:], y.ap[:, c, :], start=(c == 0), stop=(c == 7)),
                     reads=[ones_d, y], writes=[psA], pe_acc=(c > 0))
            for c in range(8):
                S.op("dve", lambda e, c=c: e.tensor_tensor(out=tbuf.ap[:, c, :], in0=y.ap[:, c, :], in1=psA.ap[:], op=ALU.subtract),
                     reads=[y, psA], writes=[tbuf])
            S.op("act", lambda e: e.activation(out=obuf.ap[:], in_=tbuf.ap[:], func=AF.Square), reads=[tbuf], writes=[obuf])

        def ln_part2(l, which, g, tbuf, obuf, psB, tmp512, rstd, final):
            for c in range(8):
                S.op("pe", lambda e, c=c: e.matmul(psB.ap[:], ones_d.ap[:], obuf.ap[:, c, :], start=(c == 0), stop=(c == 7)),
                     reads=[ones_d, obuf], writes=[psB], pe_acc=(c > 0))
            S.op("act", lambda e: e.activation(out=tmp512.ap[:], in_=psB.ap[:], func=AF.Ln, bias=LN_EPS), reads=[psB], writes=[tmp512])
            S.op("act", lambda e: e.activation(out=rstd.ap[:], in_=tmp512.ap[:], func=AF.Exp, scale=-0.5), reads=[tmp512], writes=[rstd])
            for c in range(8):
                S.op("dve", lambda e, c=c: e.tensor_tensor(out=tbuf.ap[:, c, :], in0=tbuf.ap[:, c, :], in1=rstd.ap[:], op=ALU.mult),
                     reads=[tbuf, rstd], writes=[tbuf])
            for c in range(8):
                S.op("act", lambda e, c=c: e.activation(out=obuf.ap[:, c, :], in_=tbuf.ap[:, c, :], func=AF.Identity,
                                                        scale=lncol(l, which, c), bias=lncol(l, which + 1, c)),
                     reads=[tbuf, lnp_sb], writes=[obuf])
            for c in range(8):
                S.op("act", lambda e, c=c: e.activation(out=xbf[g].ap[:, c, :], in_=tbuf.ap[:, c, :], func=AF.Identity,
                                                        scale=lncol(l, which, c), bias=lncol(l, which + 1, c)),
                     reads=[tbuf, lnp_sb], writes=[xbf[g]])
            if final:
                dst = yT_out[:, g * GT:(g + 1) * GT].rearrange("(c p) n -> p c n", p=128)
            else:
                dst = xres[:, :, g * GT:(g + 1) * GT].rearrange("c p n -> p c n")
            S.dma("sp", lambda e: e.dma_start(out=dst, in_=obuf.ap[:]), reads=[obuf])

        def phase_A(l):
            with contextlib.ExitStack() as ph:
                wt = Rot([Buf(sb(ph, "A_w%d" % i, [128, 8, 512], BF16)) for i in range(2)])
                stg = Rot([Buf(sb(ph, "A_s%d" % i, [128, 4, 512], BF16)) for i in range(3)])
                pss = Rot([Buf(ps(ph, "A_p%d" % i, [128, 512])) for i in range(6)])
                k_ev = [0]

                def evac(dst_ap, dst_buf, p, func, scale):
                    k_ev[0] += 1
                    if func is not None:
                        S.op("act", lambda e: e.activation(out=dst_ap, in_=p.ap[:], func=func), reads=[p], writes=[dst_buf])
                    elif k_ev[0] % 2 == 0:
                        S.op("act", lambda e: e.activation(out=dst_ap, in_=p.ap[:], func=AF.Copy, scale=scale), reads=[p], writes=[dst_buf])
                    else:
                        S.op("dve", lambda e: e.tensor_scalar(out=dst_ap, in0=p.ap[:], scalar1=scale, scalar2=None, op0=ALU.mult),
                             reads=[p], writes=[dst_buf])

                for cb in range(6):
                    w = wt.next()
                    S.dma("sp", lambda e, w=w, cb=cb: e.dma_start(out=w.ap[:], in_=wb_in[l][:, cb * 512:(cb + 1) * 512].rearrange("(c p) n -> p c n", p=128)),
                          writes=[w])
                    if cb in (2, 5):
                        vcol = 0 if cb == 2 else 512
                        for tt in range(8):
                            s_ = stg.next()
                            for i in range(4):
                                p = pss.next()
                                for kc in range(8):
                                    S.op("pe", lambda e, p=p, w=w, kc=kc, tt=tt, i=i: e.matmul(p.ap[:], xbf[tt].ap[:, kc, i * 128:(i + 1) * 128], w.ap[:, kc, :],
                                                                                               start=(kc == 0), stop=(kc == 7)),
                                         reads=[xbf[tt], w], writes=[p], pe_acc=(kc > 0))
                                evac(s_.ap[:, i, :], s_, p, None, 1.0)
                            S.dma("sp", lambda e, s_=s_, tt=tt, vcol=vcol: e.dma_start(
                                out=Vd[tt * 512:(tt + 1) * 512, vcol:vcol + 512].rearrange("(i p) n -> p i n", p=128), in_=s_.ap[:]), reads=[s_])
                    else:
                        if cb >= 6:
                            dst_t, cbase, func, scale = gTd, (cb - 6) * 4, AF.Sigmoid, 1.0
                        else:
                            dst_t, cbase, func = qkT, {0: 0, 1: 4, 3: 8, 4: 12}[cb], None
                            scale = 0.125 if cb in (0, 3) else 1.0
                        for g in range(NG):
                            s_ = stg.next()
                            for c in range(4):
                                p = pss.next()
                                for kc in range(8):
                                    S.op("pe", lambda e, p=p, w=w, kc=kc, g=g, c=c: e.matmul(p.ap[:], w.ap[:, kc, c * 128:(c + 1) * 128], xbf[g].ap[:, kc, :],
                                                                                             start=(kc == 0), stop=(kc == 7)),
                                         reads=[xbf[g], w], writes=[p], pe_acc=(kc > 0))
                                evac(s_.ap[:, c, :], s_, p, func, scale)
                            S.dma("sp", lambda e, s_=s_, g=g, dst_t=dst_t, cbase=cbase: e.dma_start(
                                out=dst_t[cbase:cbase + 4, :, g * GT:(g + 1) * GT].rearrange("c p n -> p c n"), in_=s_.ap[:]), reads=[s_])
                S.barrier(dummy)

        def phase_B1(l, Bm_all):
            with contextlib.ExitStack() as ph:
                qT = Rot([Buf(sb(ph, "B_q%d" % i, [128, S_LEN], BF16)) for i in range(2)])
                kT = Rot([Buf(sb(ph, "B_k%d" % i, [128, S_LEN], BF16)) for i in range(2)])
                Vt = Rot([Buf(sb(ph, "B_v%d" % i, [128, 32, 128], BF16)) for i in range(2)])
                YTs = Rot([Buf(sb(ph, "B_y%d" % i, [128, S_LEN], BF16)) for i in range(2)])
                Sb = Rot([Buf(sb(ph, "B_sb%d" % i, [128, 640], F32)) for i in range(4)])
                Pe = Rot([Buf(sb(ph, "B_pe%d" % i, [128, 640], BF16)) for i in range(8)])
                Pn = Rot([Buf(sb(ph, "B_pn%d" % i, [128, 640], BF16)) for i in range(4)])
                PnT = Rot([Buf(sb(ph, "B_pt%d" % i, [128, 5, 128], BF16)) for i in range(8)])
                stt = Rot([Buf(sb(ph, "B_st%d" % i, [128, 4], F32)) for i in range(12)])
                psS = Rot([Buf(ps(ph, "B_ps%d" % i, [128, 1024])) for i in range(2)])
                psT = Rot([Buf(ps(ph, "B_pT%d" % i, [128, 1024], BF16)) for i in range(2)])
                psY = Rot([Buf(ps(ph, "B_pY%d" % i, [128, 512])) for i in range(2)])

                for hp in range(4):
                    q_, k_, v_, y_ = qT.next(), kT.next(), Vt.next(), YTs.next()
                    S.dma("sp", lambda e, q_=q_, hp=hp: e.dma_start(out=q_.ap[:], in_=qkT[hp, :, :]), writes=[q_])
                    S.dma("sp", lambda e, k_=k_, hp=hp: e.dma_start(out=k_.ap[:], in_=qkT[4 + hp, :, :]), writes=[k_])
                    S.dma("sp", lambda e, v_=v_, hp=hp: e.dma_start(out=v_.ap[:], in_=Vd[:, hp * 128:(hp + 1) * 128].rearrange("(t p) c -> p t c", p=128)),
                          writes=[v_])
                    bms = [Bm_all[hp * 2], Bm_all[hp * 2 + 1]]

                    state = {}

                    def s1a(b):
                        nk = min(b + 1, 5)
                        W = 128 * nk
                        k0 = (b - nk + 1) * 128
                        w1 = min(W, 512)
                        hs = []
                        for hh in range(2):
                            r0, r1 = hh * 64, hh * 64 + 64
                            pS = psS.next()
                            S.op("pe", lambda e, pS=pS, r0=r0, r1=r1: e.matmul(pS.ap[:, 0:w1], q_.ap[r0:r1, b * 128:(b + 1) * 128], k_.ap[r0:r1, k0:k0 + w1], start=True, stop=True),
                                 reads=[q_, k_], writes=[pS])
                            if W > 512:
                                S.op("pe", lambda e, pS=pS, r0=r0, r1=r1: e.matmul(pS.ap[:, 512:640], q_.ap[r0:r1, b * 128:(b + 1) * 128], k_.ap[r0:r1, k0 + 512:k0 + 640], start=True, stop=True),
                                     reads=[q_, k_], writes=[pS])
                            hs.append(dict(pS=pS, sbf=Sb.next(), pe_=Pe.next(), st_=stt.next()))
                        state[b] = dict(nk=nk, W=W, c0=640 - W, hs=hs)

                    def s1b(b):
                        s_ = state[b]
                        W, c0 = s_["W"], s_["c0"]
                        for hh in range(2):
                            d = s_["hs"][hh]
                            S.op("dve", lambda e, d=d, hh=hh: e.tensor_tensor(out=d["sbf"].ap[:, 0:W], in0=d["pS"].ap[:, 0:W], in1=bms[hh].ap[:, c0:640], op=ALU.add),
                                 reads=[d["pS"], bms[hh]], writes=[d["sbf"]])
                        for hh in range(2):
                            d = s_["hs"][hh]
                            S.op("dve", lambda e, d=d: e.tensor_reduce(out=d["st_"].ap[:, 0:1], in_=d["sbf"].ap[:, 0:W], axis=AX.X, op=ALU.max, negate=True),
                                 reads=[d["sbf"]], writes=[d["st_"]])

                    def s1c(b):
                        s_ = state[b]
                        W = s_["W"]
                        for hh in range(2):
                            d = s_["hs"][hh]
                            S.op("act", lambda e, d=d: e.activation(out=d["pe_"].ap[:, 0:W], in_=d["sbf"].ap[:, 0:W], func=AF.Exp, bias=d["st_"].ap[:, 0:1], accum_out=d["st_"].ap[:, 1:2]),
                                 reads=[d["sbf"], d["st_"]], writes=[d["pe_"], d["st_"]])

                    def s2a(b):
                        s_ = state[b]
                        W = s_["W"]
                        for hh in range(2):
                            d = s_["hs"][hh]
                            S.op("dve", lambda e, d=d: e.reciprocal(out=d["st_"].ap[:, 2:3], in_=d["st_"].ap[:, 1:2]), reads=[d["st_"]], writes=[d["st_"]])
                        for hh in range(2):
                            d = s_["hs"][hh]
                            d["pn"] = Pn.next()
                            S.op("dve", lambda e, d=d: e.tensor_scalar(out=d["pn"].ap[:, 0:W], in0=d["pe_"].ap[:, 0:W], scalar1=d["st_"].ap[:, 2:3], scalar2=None, op0=ALU.mult),
                                 reads=[d["pe_"], d["st_"]], writes=[d["pn"]])

                    def s2b(b):
                        s_ = state[b]
                        nk = s_["nk"]
                        for hh in range(2):
                            d = s_["hs"][hh]
                            d["pT"] = psT.next()
                            for i in range(nk):
                                S.op("pe", lambda e, d=d, i=i: e.transpose(d["pT"].ap[:, i * 128:(i + 1) * 128], d["pn"].ap[:, i * 128:(i + 1) * 128], identb.ap[:]),
                                     reads=[d["pn"], identb], writes=[d["pT"]])

                    def s2c(b):
                        s_ = state[b]
                        W, nk = s_["W"], s_["nk"]
                        for hh in range(2):
                            d = s_["hs"][hh]
                            d["pnT"] = PnT.next()
                            S.op("act", lambda e, d=d: e.activation(out=d["pnT"].ap[:, 0:nk, :], in_=d["pT"].ap[:, 0:W].rearrange("p (i n) -> p i n", n=128), func=AF.Copy),
                                 reads=[d["pT"]], writes=[d["pnT"]])

                    def s3(b):
                        s_ = state.pop(b)
                        nk, hs = s_["nk"], s_["hs"]
                        j0 = b - nk + 1
                        pY = psY.next()
                        for hh in range(2):
                            d = hs[hh]
                            for i in range(nk):
                                S.op("pe", lambda e, d=d, i=i, hh=hh: e.matmul(pY.ap[hh * 64:(hh + 1) * 64, 0:128], v_.ap[:, j0 + i, hh * 64:(hh + 1) * 64], d["pnT"].ap[:, i, :],
                                                                               start=(i == 0), stop=(i == nk - 1)),
                                     reads=[v_, d["pnT"]], writes=[pY], pe_acc=(i > 0))
                        S.op("act", lambda e: e.activation(out=y_.ap[:, b * 128:(b + 1) * 128], in_=pY.ap[:, 0:128], func=AF.Copy), reads=[pY], writes=[y_])

                    U = 32
                    for s in range(U + 4):
                        if s < U:
                            s1a(s)
                        if 0 <= s - 2 < U:
                            s2a(s - 2)
                            s2b(s - 2)
                        if s < U:
                            s1b(s)
                        if 0 <= s - 2 < U:
                            s2c(s - 2)
                        if s < U:
                            s1c(s)
                        if 0 <= s - 4 < U:
                            s3(s - 4)
                    S.dma("sp", lambda e, y_=y_, hp=hp: e.dma_start(out=YTd[hp, :, :], in_=y_.ap[:]), reads=[y_])
                S.barrier(dummy)

        def phase_B2(l):
            with contextlib.ExitStack() as ph:
                qT = Rot([Buf(sb(ph, "C_q%d" % i, [128, S_LEN], BF16)) for i in range(2)])
                kT = Rot([Buf(sb(ph, "C_k%d" % i, [128, S_LEN], BF16)) for i in range(2)])
                Vt = Rot([Buf(sb(ph, "C_v%d" % i, [128, 32, 128], BF16)) for i in range(2)])
                YTs = Rot([Buf(sb(ph, "C_y%d" % i, [128, S_LEN], BF16)) for i in range(2)])
                NB = 10
                Eb = Rot([Buf(sb(ph, "C_e%d" % i, [128, 512], BF16)) for i in range(NB)])
                SPb = Rot([Buf(sb(ph, "C_s%d" % i, [128, 512], BF16)) for i in range(NB)])
                Xb = Rot([Buf(sb(ph, "C_x%d" % i, [128, 512], BF16)) for i in range(NB)])
                Ab = Rot([Buf(sb(ph, "C_a%d" % i, [128, 512], BF16)) for i in range(NB)])
                psZ = Rot([Buf(ps(ph, "C_pz%d" % i, [128, 512])) for i in range(3)])
                psP = [Buf(ps(ph, "C_pp%d" % i, [128, 512])) for i in range(2)]
                psY = Buf(ps(ph, "C_pY", [128, 512]))
                gw = Buf(sb(ph, "C_gw", [128, 8, 512], BF16))
                gst = Rot([Buf(sb(ph, "C_gs%d" % i, [128, 4, 512], BF16)) for i in range(2)])
                gps = Rot([Buf(ps(ph, "C_gp%d" % i, [128, 512])) for i in range(2)])

                def gate_units():
                    for cb in range(6, 10):
                        S.dma("sp", lambda e, cb=cb: e.dma_start(out=gw.ap[:], in_=wb_in[l][:, cb * 512:(cb + 1) * 512].rearrange("(c p) n -> p c n", p=128)), writes=[gw])
                        for g in range(NG):
                            st_ = gst.next()
                            for c in range(4):
                                p = gps.next()
                                for kc in range(8):
                                    S.op("pe", lambda e, p=p, kc=kc, g=g, c=c: e.matmul(p.ap[:], gw.ap[:, kc, c * 128:(c + 1) * 128], xbf[g].ap[:, kc, :], start=(kc == 0), stop=(kc == 7)),
                                         reads=[xbf[g], gw], writes=[p], pe_acc=(kc > 0))
                                    if kc % 2 == 1 and kc < 7:
                                        yield
                                S.op("dve", lambda e, p=p, st_=st_, c=c: e.tensor_copy(out=st_.ap[:, c, :], in_=p.ap[:]), reads=[p], writes=[st_])
                                if c == 3:
                                    S.dma("sp", lambda e, st_=st_, g=g, cb=cb: e.dma_start(
                                        out=gTd[(cb - 6) * 4:(cb - 6) * 4 + 4, :, g * GT:(g + 1) * GT].rearrange("c p n -> p c n"), in_=st_.ap[:]), reads=[st_])
                                yield
                gate_gen = gate_units()
                cgen = cast_gen(l + 1) if l + 1 < n_layers else iter(())

                for hp in range(4):
                    q_, k_, v_, y_ = qT.next(), kT.next(), Vt.next(), YTs.next()
                    S.dma("sp", lambda e, q_=q_, hp=hp: e.dma_start(out=q_.ap[:], in_=qkT[8 + hp, :, :]), writes=[q_])
                    S.dma("sp", lambda e, k_=k_, hp=hp: e.dma_start(out=k_.ap[:], in_=qkT[12 + hp, :, :]), writes=[k_])
                    S.dma("sp", lambda e, v_=v_, hp=hp: e.dma_start(out=v_.ap[:], in_=Vd[:, 512 + hp * 128:512 + (hp + 1) * 128].rearrange("(t p) c -> p t c", p=128)),
                          writes=[v_])
                    steps = [(G, j) for G in range(8) for j in range(4 * G + 3, -1, -1)]
                    state = {}
                    prev_sp = {0: None, 1: None}

                    def stage1(u):
                        G, j = steps[u]
                        o = j - 4 * G
                        qs = max(o, 0) * 128
                        st_u = []
                        for hh in range(2):
                            r0, r1 = hh * 64, hh * 64 + 64
                            pz, eb, sp = psZ.next(), Eb.next(), SPb.next()
                            S.op("pe", lambda e, pz=pz, r0=r0, r1=r1: e.matmul(pz.ap[:, qs:512], k_.ap[r0:r1, j * 128:(j + 1) * 128], q_.ap[r0:r1, G * 512 + qs:(G + 1) * 512],
                                                                               start=True, stop=True), reads=[q_, k_], writes=[pz])
                            st_u.append(dict(pz=pz, eb=eb, sp=sp))
                        for hh in range(2):
                            d = st_u[hh]
                            S.op("act", lambda e, d=d: e.activation(out=d["eb"].ap[:, qs:512], in_=d["pz"].ap[:, qs:512], func=AF.Exp), reads=[d["pz"]], writes=[d["eb"]])
                        for hh in range(2):
                            d = st_u[hh]
                            S.op("act", lambda e, d=d: e.activation(out=d["sp"].ap[:, qs:512], in_=d["eb"].ap[:, qs:512], func=AF.Ln, bias=1.0), reads=[d["eb"]], writes=[d["sp"]])
                        if o >= 0:
                            for hh in range(2):
                                d = st_u[hh]
                                S.op("pool", lambda e, d=d: e.tensor_tensor(out=d["sp"].ap[:, qs:qs + 128], in0=d["sp"].ap[:, qs:qs + 128], in1=trimask.ap[:], op=ALU.mult),
                                     reads=[d["sp"], trimask], writes=[d["sp"]])
                        state[u] = dict(G=G, j=j, o=o, qs=qs, hs=st_u)

                    def stage2(u):
                        s_ = state[u]
                        G, j, o, qs = s_["G"], s_["j"], s_["o"], s_["qs"]
                        first = (j == 4 * G + 3)
                        for hh in range(2):
                            d = s_["hs"][hh]
                            if not first:
                                psp, pqs = prev_sp[hh]
                                S.op("pe", lambda e, hh=hh, psp=psp, pqs=pqs: e.matmul(psP[hh].ap[:, pqs:512], nstr.ap[:], psp.ap[:, pqs:512], start=False, stop=False, skip_group_check=True),
                                     reads=[nstr, psp], writes=[psP[hh]], pe_acc=True)
                            S.op("pe", lambda e, hh=hh, d=d: e.matmul(psP[hh].ap[:, qs:512], ntri.ap[:], d["sp"].ap[:, qs:512], start=first, stop=True, skip_group_check=True),
                                 reads=[ntri, d["sp"]], writes=[psP[hh]], pe_acc=(not first))
                            prev_sp[hh] = (d["sp"], qs)
                        for hh in range(2):
                            d = s_["hs"][hh]
                            d["xb"] = Xb.next()
                            S.op("act", lambda e, hh=hh, d=d: e.activation(out=d["xb"].ap[:, qs:512], in_=psP[hh].ap[:, qs:512], func=AF.Exp), reads=[psP[hh]], writes=[d["xb"]])
                        for hh in range(2):
                            d = s_["hs"][hh]
                            d["ab"] = Ab.next()
                            S.op("dve", lambda e, d=d: e.tensor_tensor(out=d["ab"].ap[:, qs:512], in0=d["eb"].ap[:, qs:512], in1=d["xb"].ap[:, qs:512], op=ALU.mult),
                                 reads=[d["eb"], d["xb"]], writes=[d["ab"]])
                        if o >= 0:
                            for hh in range(2):
                                d = s_["hs"][hh]
                                S.op("pool", lambda e, d=d: e.tensor_tensor(out=d["ab"].ap[:, qs:qs + 128], in0=d["ab"].ap[:, qs:qs + 128], in1=trimask.ap[:], op=ALU.mult),
                                     reads=[d["ab"], trimask], writes=[d["ab"]])

                    def stage3(u):
                        s_ = state.pop(u)
                        G, j, qs = s_["G"], s_["j"], s_["qs"]
                        first = (j == 4 * G + 3)
                        for hh in range(2):
                            d = s_["hs"][hh]
                            S.op("pe", lambda e, hh=hh, d=d: e.matmul(psY.ap[hh * 64:(hh + 1) * 64, qs:512], v_.ap[:, j, hh * 64:(hh + 1) * 64], d["ab"].ap[:, qs:512],
                                                                      start=first, stop=(j == 0), skip_group_check=True),
                                 reads=[v_, d["ab"]], writes=[psY], pe_acc=(not first))
                        if j == 0:
                            S.op("dve", lambda e: e.tensor_copy(out=y_.ap[:, G * 512:(G + 1) * 512], in_=psY.ap[:]), reads=[psY], writes=[y_])

                    U = len(steps)
                    for s in range(U + 4):
                        if s < U:
                            stage1(s)
                        if 0 <= s - 2 < U:
                            stage2(s - 2)
                        if 0 <= s - 4 < U:
                            stage3(s - 4)
                        next(gate_gen, None)
                        if s % 6 == 3:
                            next(cgen, None)
                    S.dma("sp", lambda e, y_=y_, hp=hp: e.dma_start(out=YTd[4 + hp, :, :], in_=y_.ap[:]), reads=[y_])
                for _ in gate_gen:
                    pass
                for _ in cgen:
                    pass
                S.barrier(dummy)

        def phase_C(l):
            with contextlib.ExitStack() as ph:
                wa = Buf(sb(ph, "D_wa", [128, 4, D], BF16))
                wbb = Buf(sb(ph, "D_wb", [128, 4, D], BF16))
                wo = Buf(sb(ph, "D_wo", [128, 8, D], BF16))
                Yin = Rot([Buf(sb(ph, "D_y%d" % i, [128, 8, GT], BF16)) for i in range(1)])
                Gin = Rot([Buf(sb(ph, "D_g%d" % i, [128, 16, GT], BF16)) for i in range(1)])
                bufA = Buf(sb(ph, "D_bA", [128, 8, GT], F32))
                bufB = Buf(sb(ph, "D_bB", [128, 8, GT], F32))
                bufC = Buf(sb(ph, "D_bC", [128, 8, GT], F32))
                mrg = Buf(sb(ph, "D_m", [128, 8, GT], BF16))
                t1 = Rot([Buf(sb(ph, "D_t1%d" % i, [128, GT], BF16)) for i in range(2)])
                t2 = Rot([Buf(sb(ph, "D_t2%d" % i, [128, GT], BF16)) for i in range(2)])
                tmp512 = Buf(sb(ph, "D_tmp", [128, GT], F32))
                rstd = Buf(sb(ph, "D_rstd", [128, GT], F32))
                psa = Rot([Buf(ps(ph, "D_pa%d" % i, [128, 512])) for i in range(3)])
                psb = psa
                psR = Buf(ps(ph, "D_pR", [128, 512]))
                pso = Rot([Buf(ps(ph, "D_po%d" % i, [128, 512])) for i in range(1)])
                psTk = Buf(ps(ph, "D_pT", [128, 1024], BF16))
                tkst = Rot([Buf(sb(ph, "D_tk%d" % i, [128, D], BF16)) for i in range(2)])
                psA = Buf(ps(ph, "D_pA", [128, 512]))
                psB = Buf(ps(ph, "D_pB", [128, 512]))
                S.dma("sp", lambda e: e.dma_start(out=wa.ap[:], in_=wb_upa[l].rearrange("(c p) n -> p c n", p=128)), writes=[wa])
                S.dma("sp", lambda e: e.dma_start(out=wbb.ap[:], in_=wb_upb[l].rearrange("(c p) n -> p c n", p=128)), writes=[wbb])
                S.dma("sp", lambda e: e.dma_start(out=wo.ap[:], in_=wb_out[l].rearrange("(c p) n -> p c n", p=128)), writes=[wo])
                xsrc = (lambda g: xT_in[:, g * GT:(g + 1) * GT].rearrange("(c p) n -> p c n", p=128)) if l == 0 else \
                       (lambda g: xres[:, :, g * GT:(g + 1) * GT].rearrange("c p n -> p c n"))

                def loads(g):
                    y_ = Yin.next()
                    S.dma("sp", lambda e: e.dma_start(out=y_.ap[:], in_=YTd[:, :, g * GT:(g + 1) * GT].rearrange("c p n -> p c n")), writes=[y_])
                    return y_

                pre = {}

                def preload(g):
                    y_ = loads(g)
                    g_ = Gin.next()
                    S.dma("sp", lambda e, g=g, g_=g_: e.dma_start(out=g_.ap[:], in_=gTd[:, :, g * GT:(g + 1) * GT].rearrange("c p n -> p c n")), writes=[g_])
                    S.op("act", lambda e, g_=g_: e.activation(out=g_.ap[:], in_=g_.ap[:], func=AF.Sigmoid), reads=[g_], writes=[g_])
                    pre[g] = (y_, g_)

                def G1(g):
                    y_, g_ = pre.pop(g)
                    for dc in range(8):
                        pa, pb = psa.next(), psb.next()
                        for kc in range(4):
                            S.op("pe", lambda e, pa=pa, kc=kc, dc=dc: e.matmul(pa.ap[:], wa.ap[:, kc, dc * 128:(dc + 1) * 128], y_.ap[:, kc, :], start=(kc == 0), stop=(kc == 3)),
                                 reads=[wa, y_], writes=[pa], pe_acc=(kc > 0))
                        for kc in range(4):
                            S.op("pe", lambda e, pb=pb, kc=kc, dc=dc: e.matmul(pb.ap[:], wbb.ap[:, kc, dc * 128:(dc + 1) * 128], y_.ap[:, 4 + kc, :], start=(kc == 0), stop=(kc == 3)),
                                 reads=[wbb, y_], writes=[pb], pe_acc=(kc > 0))
                        a1, a2 = t1.next(), t2.next()
                        S.op("dve", lambda e, a1=a1, pa=pa, dc=dc: e.tensor_tensor(out=a1.ap[:], in0=g_.ap[:, dc, :], in1=pa.ap[:], op=ALU.mult), reads=[g_, pa], writes=[a1])
                        S.op("dve", lambda e, a2=a2, pb=pb, dc=dc: e.tensor_tensor(out=a2.ap[:], in0=g_.ap[:, 8 + dc, :], in1=pb.ap[:], op=ALU.mult), reads=[g_, pb], writes=[a2])
                        S.op("dve", lambda e, a1=a1, a2=a2, dc=dc: e.tensor_tensor(out=mrg.ap[:, dc, :], in0=a1.ap[:], in1=a2.ap[:], op=ALU.add), reads=[a1, a2], writes=[mrg])
                    if g + 1 < NG:
                        preload(g + 1)

                def xload(g):
                    S.dma("sp", lambda e, g=g: e.dma_start(out=bufA.ap[:], in_=xsrc(g)), writes=[bufA])

                def G2(g):
                    for dc in range(8):
                        po = pso.next()
                        for kc in range(8):
                            S.op("pe", lambda e, po=po, kc=kc, dc=dc: e.matmul(po.ap[:], wo.ap[:, kc, dc * 128:(dc + 1) * 128], mrg.ap[:, kc, :], start=(kc == 0), stop=(kc == 7)),
                                 reads=[wo, mrg], writes=[po], pe_acc=(kc > 0))
                        S.op("dve", lambda e, po=po, dc=dc: e.scalar_tensor_tensor(out=bufB.ap[:, dc, :], in0=bufA.ap[:, dc, :], scalar=ALPHA, in1=po.ap[:], op0=ALU.mult, op1=ALU.add),
                             reads=[bufA, po], writes=[bufB])
                    if g + 1 < NG:
                        xload(g + 1)

                def TK(g):
                    for sub in range(4):
                        for c in range(8):
                            S.op("pe", lambda e, sub=sub, c=c: e.transpose(psTk.ap[:, c * 128:(c + 1) * 128], xbf[g].ap[:, c, sub * 128:(sub + 1) * 128], identb.ap[:]),
                                 reads=[xbf[g], identb], writes=[psTk])
                        tk = tkst.next()
                        if sub % 2 == 0:
                            S.op("act", lambda e, tk=tk: e.activation(out=tk.ap[:], in_=psTk.ap[:], func=AF.Copy), reads=[psTk], writes=[tk])
                        else:
                            S.op("dve", lambda e, tk=tk: e.tensor_copy(out=tk.ap[:], in_=psTk.ap[:]), reads=[psTk], writes=[tk])
                        S.dma("sp", lambda e, tk=tk, sub=sub, g=g: e.dma_start(out=x1tok[g * GT + sub * 128:g * GT + (sub + 1) * 128, :], in_=tk.ap[:]), reads=[tk])

                def LN(g, part):
                    layer_norm(ph, l, 0, g, bufB, bufB, bufC, psA, psB, tmp512, rstd, final=False, part=part)

                def RT(g):
                    for ii in range(4):
                        i = g * 4 + ii
                        for c in range(8):
                            S.op("pe", lambda e, ii=ii, c=c, i=i: e.matmul(psR.ap[:, i * 16:(i + 1) * 16], bufC.ap[:, c, ii * 128:(ii + 1) * 128], wr32.ap[:, c, :], start=(c == 0), stop=(c == 7)),
                                 reads=[bufC, wr32], writes=[psR], pe_acc=(c > 0))

                preload(0)
                xload(0)
                G1(0)
                G2(0)
                LN(0, 1)
                for g in range(1, NG):
                    LN(g - 1, 2)
                    G1(g)
                    G2(g)
                    RT(g - 1)
                    LN(g, 1)
                    TK(g - 1)
                LN(NG - 1, 2)
                RT(NG - 1)
                TK(NG - 1)
                S.op("dve", lambda e: e.tensor_tensor(out=lg_glob.ap[:], in0=psR.ap[:].rearrange("p (t e) -> p t e", e=NE), in1=brb_sb.ap[:].unsqueeze(1).to_broadcast([128, 32, NE]), op=ALU.add),
                     reads=[psR, brb_sb], writes=[lg_glob])
                S.barrier(dummy)

        def phase_D(l, final):
            with contextlib.ExitStack() as pz:
                NT = 32
                sel_all = Buf(sb(pz, "R_sel", [128, NT, NE], F32))
                m1_all = Buf(sb(pz, "R_m1", [128, NT, NE], F32))
                comb_all = Buf(sb(pz, "R_cmb", [128, NT, NE], F32))
                rank_all = Buf(sb(pz, "R_rank", [128, NT, NE], F32))
                tmp_all = Buf(sb(pz, "R_tmp", [128, NT, NE], F32))
                posw = Buf(sb(pz, "R_posw", [128, 4, NT], F32))
                idxAB = Buf(sb(pz, "R_idx", [128, 2, NT], I32))
                rowsAB = Buf(sb(pz, "R_rows", [128, 2, NT, 2], F32))
                widx_f = Buf(sb(pz, "R_wf", [128, NTL], F32))
                widx = Buf(sb(pz, "R_wi", [128, NTL], I32))
                tmpT = Buf(sb(pz, "R_tmpT", [128, NTL, NE], F32))
                sm16 = Buf(sb(pz, "R_sm", [128, 8 * NE], F32))
                with contextlib.ExitStack() as ph:
                    ex_all = Buf(sb(ph, "R_ex", [128, NT, NE], F32))
                    em_all = Buf(sb(ph, "R_em", [128, NT, NE], F32))
                    selb_all = Buf(sb(ph, "R_selb", [128, NT, NE], BF16))
                    cum_a = Buf(sb(ph, "R_cuma", [128, NT, NE], F32))
                    cum_b = Buf(sb(ph, "R_cumb", [128, NT, NE], F32))
                    cumb_bf = Buf(sb(ph, "R_cumbf", [128, NT, NE], BF16))
                    ssum = Buf(sb(ph, "R_ssum", [128, NE], F32))
                    ssum_bf = Buf(sb(ph, "R_ssumb", [128, NE], BF16))
                    g4 = Buf(sb(ph, "R_g4", [128, NT, 4], F32))
                    v32 = Buf(sb(ph, "R_v32", [128, 6, NT], F32))
                    psK = Rot([Buf(ps(ph, "R_pk%d" % i, [128, 512])) for i in range(2)])
                    slot_z = Buf(slotmap, "slot_z")
                    S.dma("sp", lambda e: e.dma_start(out=slotmap.rearrange("(p a) b -> p (a b)", p=128), in_=zeros.ap[:, 0:NSLOT * 2 // 128]), reads=[zeros], writes=[slot_z])
                    mx, gm, top1, top2, den = (v32.ap[:, k, :] for k in range(5))
                    bc16 = lambda ap: ap.unsqueeze(2).to_broadcast([128, NT, NE])
                    v3 = lambda buf: buf.ap[:]
                    g44 = lambda buf: buf.ap[:].rearrange("p t (a b) -> p (t a) b", b=4)
                    S.op("dve", lambda e: e.tensor_reduce(out=mx, in_=lg_glob.ap[:], axis=AX.X, op=ALU.max), reads=[lg_glob], writes=[v32])
                    S.op("dve", lambda e: e.tensor_tensor(out=ex_all.ap[:], in0=lg_glob.ap[:], in1=bc16(mx), op=ALU.subtract), reads=[lg_glob, v32], writes=[ex_all])
                    S.op("act", lambda e: e.activation(out=ex_all.ap[:], in_=ex_all.ap[:], func=AF.Exp), reads=[ex_all], writes=[ex_all])
                    S.op("dve", lambda e: e.tensor_reduce(out=g4.ap[:].rearrange("p t a -> p (t a)"), in_=g44(ex_all), axis=AX.X, op=ALU.max), reads=[ex_all], writes=[g4])
                    S.op("dve", lambda e: e.tensor_reduce(out=gm, in_=g4.ap[:], axis=AX.X, op=ALU.max), reads=[g4], writes=[v32])
                    S.op("dve", lambda e: e.tensor_tensor(out=g4.ap[:], in0=g4.ap[:], in1=gm.unsqueeze(2).to_broadcast([128, NT, 4]), op=ALU.is_ge), reads=[g4, v32], writes=[g4])
                    S.op("dve", lambda e: e.tensor_tensor(out=g44(em_all), in0=g44(ex_all), in1=g4.ap[:].rearrange("p t a -> p (t a)").unsqueeze(2).to_broadcast([128, NT * 4, 4]), op=ALU.mult),
                         reads=[ex_all, g4], writes=[em_all])
                    S.op("dve", lambda e: e.tensor_reduce(out=top1, in_=em_all.ap[:], axis=AX.X, op=ALU.max), reads=[em_all], writes=[v32])
                    S.op("dve", lambda e: e.tensor_tensor(out=m1_all.ap[:], in0=em_all.ap[:], in1=bc16(top1), op=ALU.is_ge), reads=[em_all, v32], writes=[m1_all])
                    S.op("dve", lambda e: e.tensor_tensor(out=tmp_all.ap[:], in0=em_all.ap[:], in1=m1_all.ap[:], op=ALU.mult), reads=[em_all, m1_all], writes=[tmp_all])
                    S.op("dve", lambda e: e.tensor_tensor(out=em_all.ap[:], in0=em_all.ap[:], in1=tmp_all.ap[:], op=ALU.subtract), reads=[em_all, tmp_all], writes=[em_all])
                    S.op("dve", lambda e: e.tensor_reduce(out=top2, in_=em_all.ap[:], axis=AX.X, op=ALU.max), reads=[em_all], writes=[v32])
                    S.op("dve", lambda e: e.tensor_tensor(out=sel_all.ap[:], in0=em_all.ap[:], in1=bc16(top2), op=ALU.is_ge), reads=[em_all, v32], writes=[sel_all])
                    S.op("dve", lambda e: e.tensor_tensor(out=comb_all.ap[:], in0=sel_all.ap[:], in1=m1_all.ap[:], op=ALU.add), reads=[sel_all, m1_all], writes=[comb_all])
                    S.op("dve", lambda e: e.tensor_tensor(out=den, in0=top1, in1=top2, op=ALU.add), reads=[v32], writes=[v32])
                    S.op("dve", lambda e: e.reciprocal(out=den, in_=den), reads=[v32], writes=[v32])
                    S.op("dve", lambda e: e.tensor_tensor(out=posw.ap[:, 2, :], in0=top1, in1=den, op=ALU.mult), reads=[v32], writes=[posw])
                    S.op("dve", lambda e: e.tensor_tensor(out=posw.ap[:, 3, :], in0=top2, in1=den, op=ALU.mult), reads=[v32], writes=[posw])
                    S.op("act", lambda e: e.activation(out=selb_all.ap[:], in_=comb_all.ap[:], func=AF.Copy), reads=[comb_all], writes=[selb_all])
                    S.op("dve", lambda e: e.memset(cum_a.ap[:, 0:1, :], 0.0), writes=[cum_a])
                    S.op("dve", lambda e: e.tensor_copy(out=cum_a.ap[:, 1:NT, :], in_=comb_all.ap[:, 0:NT - 1, :]), reads=[comb_all], writes=[cum_a])
                    ca, cb_ = cum_a, cum_b
                    for sh in (1, 2, 4, 8, 16):
                        S.op("dve", lambda e, ca=ca, cb_=cb_, sh=sh: e.tensor_copy(out=cb_.ap[:, 0:sh, :], in_=ca.ap[:, 0:sh, :]), reads=[ca], writes=[cb_])
                        S.op("dve", lambda e, ca=ca, cb_=cb_, sh=sh: e.tensor_tensor(out=cb_.ap[:, sh:NT, :], in0=ca.ap[:, sh:NT, :], in1=ca.ap[:, 0:NT - sh, :], op=ALU.add), reads=[ca], writes=[cb_])
                        ca, cb_ = cb_, ca
                    S.op("act", lambda e, ca=ca: e.activation(out=cumb_bf.ap[:], in_=ca.ap[:], func=AF.Copy), reads=[ca], writes=[cumb_bf])
                    S.op("dve", lambda e, ca=ca: e.tensor_tensor(out=ssum.ap[:], in0=ca.ap[:, NT - 1, :], in1=comb_all.ap[:, NT - 1, :], op=ALU.add), reads=[ca, comb_all], writes=[ssum])
                    S.op("dve", lambda e: e.tensor_copy(out=ssum_bf.ap[:], in_=ssum.ap[:]), reads=[ssum], writes=[ssum_bf])
                    pk = psK.next()
                    S.op("pe", lambda e, pk=pk: e.matmul(pk.ap[:], pstr.ap[:], selb_all.ap[:].rearrange("p t e -> p (t e)"), start=True, stop=False), reads=[pstr, selb_all], writes=[pk])
                    S.op("pe", lambda e, pk=pk: e.matmul(pk.ap[:], pones.ap[:], cumb_bf.ap[:].rearrange("p t e -> p (t e)"), start=False, stop=True), reads=[pones, cumb_bf], writes=[pk], pe_acc=True)
                    S.op("act", lambda e, pk=pk: e.activation(out=rank_all.ap[:], in_=pk.ap[:].rearrange("p (t e) -> p t e", e=NE), func=AF.Copy), reads=[pk], writes=[rank_all])
                    cfin = ssum_bf
                    pk = psK.next()
                    S.op("pe", lambda e: e.matmul(pk.ap[:, 0:16], pones.ap[:], cfin.ap[:], start=True, stop=True), reads=[pones, cfin], writes=[pk])
                    n_ = sm16.ap[:, 0:16]
                    T_ = sm16.ap[:, 16:32]
                    sc = [sm16.ap[:, 32:48], sm16.ap[:, 48:64]]
                    base_ = sm16.ap[:, 64:80]
                    S.op("dve", lambda e: e.tensor_copy(out=n_, in_=pk.ap[:, 0:16]), reads=[pk], writes=[sm16])
                    S.op("dve", lambda e: e.tensor_scalar(out=T_, in0=n_, scalar1=0.0, scalar2=None, op0=ALU.is_gt), reads=[sm16], writes=[sm16])
                    for m in range(1, S_LEN // TS):
                        S.op("dve", lambda e, m=m: e.scalar_tensor_tensor(out=T_, in0=n_, scalar=float(TS) * m, in1=T_, op0=ALU.is_gt, op1=ALU.add), reads=[sm16], writes=[sm16])
                    S.op("dve", lambda e: e.tensor_copy(out=sc[0], in_=T_), reads=[sm16], writes=[sm16])
                    cur = 0
                    for sh in (1, 2, 4, 8):
                        a, b_ = sc[cur], sc[1 - cur]
                        S.op("dve", lambda e, a=a, b_=b_, sh=sh: e.tensor_copy(out=b_[:, 0:sh], in_=a[:, 0:sh]), reads=[sm16], writes=[sm16])
                        S.op("dve", lambda e, a=a, b_=b_, sh=sh: e.tensor_tensor(out=b_[:, sh:16], in0=a[:, sh:16], in1=a[:, 0:16 - sh], op=ALU.add), reads=[sm16], writes=[sm16])
                        cur = 1 - cur
                    cumT = sc[cur]
                    S.op("dve", lambda e: e.tensor_tensor(out=base_, in0=cumT, in1=T_, op=ALU.subtract), reads=[sm16], writes=[sm16])
                    S.op("dve", lambda e: e.tensor_scalar(out=base_, in0=base_, scalar1=float(TS), scalar2=None, op0=ALU.mult), reads=[sm16], writes=[sm16])
                    S.op("dve", lambda e: e.tensor_tensor(out=rank_all.ap[:], in0=rank_all.ap[:], in1=base_.unsqueeze(1).to_broadcast([128, NT, NE]), op=ALU.add),
                         reads=[rank_all, sm16], writes=[rank_all])
                    for k, (src, msk) in enumerate(((rank_all, m1_all), (rank_all, sel_all))):
                        S.op("dve", lambda e, src=src, msk=msk: e.tensor_tensor(out=tmp_all.ap[:], in0=src.ap[:], in1=msk.ap[:], op=ALU.mult), reads=[src, msk], writes=[tmp_all])
                        S.op("dve", lambda e, k=k: e.tensor_reduce(out=posw.ap[:, k, :], in_=tmp_all.ap[:], axis=AX.X, op=ALU.add), reads=[tmp_all], writes=[posw])
                    S.op("dve", lambda e: e.tensor_copy(out=idxAB.ap[:], in_=posw.ap[:, 0:2, :]), reads=[posw], writes=[idxAB])
                    for k in range(2):
                        S.op("pool", lambda e, k=k: e.tensor_copy(out=rowsAB.ap[:, k, :, 0], in_=tokid.ap[:]), reads=[tokid], writes=[rowsAB])
                        S.op("pool", lambda e, k=k: e.tensor_copy(out=rowsAB.ap[:, k, :, 1], in_=posw.ap[:, 2 + k, :]), reads=[posw], writes=[rowsAB])
                    S.op("dve", lambda e: e.tensor_tensor(out=tmpT.ap[:], in0=tvals.ap[:], in1=cumT.unsqueeze(1).to_broadcast([128, NTL, NE]), op=ALU.is_ge),
                         reads=[tvals, sm16], writes=[tmpT])
                    S.op("dve", lambda e: e.tensor_reduce(out=widx_f.ap[:], in_=tmpT.ap[:], axis=AX.X, op=ALU.add), reads=[tmpT], writes=[widx_f])
                    S.op("dve", lambda e: e.tensor_scalar(out=widx_f.ap[:], in0=widx_f.ap[:], scalar1=15.0, scalar2=128.0, op0=ALU.min, op1=ALU.mult), reads=[widx_f], writes=[widx_f])
                    S.op("dve", lambda e: e.tensor_scalar(out=widx_f.ap[:], in0=widx_f.ap[:], scalar1=pidx.ap[:, 0:1], scalar2=None, op0=ALU.add), reads=[widx_f, pidx], writes=[widx_f])
                    S.op("dve", lambda e: e.tensor_copy(out=widx.ap[:], in_=widx_f.ap[:]), reads=[widx_f], writes=[widx])
                    for k in range(2):
                        for i in range(NT):
                            S.dma("pool", lambda e, k=k, i=i: e.indirect_dma_start(out=slotmap[:, :], out_offset=bass.IndirectOffsetOnAxis(ap=idxAB.ap[:, k, i:i + 1], axis=0),
                                                                                   in_=rowsAB.ap[:, k, i, :], in_offset=None),
                                  reads=[idxAB, rowsAB, slot_z])
                    S.barrier(dummy)
                with contextlib.ExitStack() as ph:
                    wg = Rot([Buf(sb(ph, "E_wg%d" % i, [128, 8, DF], BF16)) for i in range(2)])
                    wu = Rot([Buf(sb(ph, "E_wu%d" % i, [128, 8, DF], BF16)) for i in range(2)])
                    wd = Rot([Buf(sb(ph, "E_wd%d" % i, [128, 4, D], BF16)) for i in range(2)])
                    small = Buf(sb(ph, "E_sm", [128, NTL, SUB, 2], F32))
                    tkall = Buf(sb(ph, "E_ti", [128, NTL, SUB], I32))
                    xg_t = [sb(ph, "E_xg%d" % i, [128, SUB, D], BF16) for i in range(3)]
                    xg = Rot([[Buf(t_[:, s_, :]) for s_ in range(SUB)] for t_ in xg_t])
                    sm_ld = []
                    for t0_ in range(0, NTL, 8):
                        S.dma("sp", lambda e, t0_=t0_: e.dma_start(out=small.ap[:, t0_:t0_ + 8, :, :],
                                                                   in_=slotmap[t0_ * TS:(t0_ + 8) * TS, :].rearrange("(t s p) b -> p t s b", p=128, s=SUB)), writes=[small])
                    S.op("dve", lambda e: e.tensor_copy(out=tkall.ap[:], in_=small.ap[:, :, :, 0]), reads=[small], writes=[tkall])
                    xgT = Rot([Buf(sb(ph, "E_xT%d" % i, [128, 8, TS], BF16)) for i in range(2)])
                    hb = Rot([Buf(sb(ph, "E_h%d" % i, [128, 4, TS], BF16)) for i in range(2)])
                    s1b = Rot([Buf(sb(ph, "E_s%d" % i, [128, TS], BF16)) for i in range(3)])
                    ost = Rot([Buf(sb(ph, "E_o%d" % i, [128, SUB, D], F32)) for i in range(3)])
                    psT = Rot([Buf(ps(ph, "E_pT%d" % i, [128, 512])) for i in range(2)])
                    psg = Rot([Buf(ps(ph, "E_pg%d" % i, [128, 512])) for i in range(2)])
                    psu = Rot([Buf(ps(ph, "E_pu%d" % i, [128, 512])) for i in range(2)])
                    pso = Rot([Buf(ps(ph, "E_po%d" % i, [128, 512])) for i in range(2)])

                    def pre_w(t):
                        a, b_, c_ = wg.next(), wu.next(), wd.next()
                        for dst, srcw in ((a, wb_g[l]), (b_, wb_u[l]), (c_, wb_d[l])):
                            S.dma("pool", lambda e, dst=dst, srcw=srcw, t=t: e.indirect_dma_start(out=dst.ap[:].rearrange("p c n -> p (c n)"), out_offset=None, in_=srcw[:, :],
                                                                                                in_offset=bass.IndirectOffsetOnAxis(ap=widx.ap[:, t:t + 1], axis=0)),
                                  reads=[widx], writes=[dst])
                        return a, b_, c_

                    def pre_x(t):
                        x_ = xg.next()
                        for s_ in range(SUB):
                            S.dma("pool", lambda e, x_=x_, t=t, s_=s_: e.indirect_dma_start(out=x_[s_].ap, out_offset=None, in_=x1tok[:, :],
                                                                                            in_offset=bass.IndirectOffsetOnAxis(ap=tkall.ap[:, t, s_:s_ + 1], axis=0)),
                                  reads=[tkall], writes=[x_[s_]])
                        return x_

                    kev = [0]
                    KP = 512 // TS

                    def TT(t, x_):
                        xT = xgT.next()
                        for k0 in range(0, 8, KP):
                            pT = psT.next()
                            for kk in range(KP):
                                kc = k0 + kk
                                for s_ in range(SUB):
                                    S.op("pe", lambda e, pT=pT, kc=kc, kk=kk, s_=s_: e.matmul(pT.ap[:, kk * TS + s_ * 128:kk * TS + (s_ + 1) * 128], x_[s_].ap[:, kc * 128:(kc + 1) * 128], identb.ap[:], start=True, stop=True),
                                         reads=[x_[s_], identb], writes=[pT])
                            kev[0] += 1
                            if kev[0] % 2 == 0:
                                S.op("act", lambda e, pT=pT, k0=k0: e.activation(out=xT.ap[:, k0:k0 + KP, :], in_=pT.ap[:].rearrange("p (k n) -> p k n", k=KP), func=AF.Copy), reads=[pT], writes=[xT])
                            else:
                                S.op("dve", lambda e, pT=pT, k0=k0: e.tensor_copy(out=xT.ap[:, k0:k0 + KP, :], in_=pT.ap[:].rearrange("p (k n) -> p k n", k=KP)), reads=[pT], writes=[xT])
                        return xT

                    def GU(t, a, b_, xT):
                        h = hb.next()
                        for fc in range(4):
                            pg, pu = psg.next(), psu.next()
                            for kc in range(8):
                                S.op("pe", lambda e, pg=pg, kc=kc, fc=fc: e.matmul(pg.ap[:, 0:TS], a.ap[:, kc, fc * 128:(fc + 1) * 128], xT.ap[:, kc, :], start=(kc == 0), stop=(kc == 7)),
                                     reads=[a, xT], writes=[pg], pe_acc=(kc > 0))
                            for kc in range(8):
                                S.op("pe", lambda e, pu=pu, kc=kc, fc=fc: e.matmul(pu.ap[:, 0:TS], b_.ap[:, kc, fc * 128:(fc + 1) * 128], xT.ap[:, kc, :], start=(kc == 0), stop=(kc == 7)),
                                     reads=[b_, xT], writes=[pu], pe_acc=(kc > 0))
                            s1 = s1b.next()
                            S.op("act", lambda e, pg=pg, s1=s1: e.activation(out=s1.ap[:], in_=pg.ap[:, 0:TS], func=AF.Silu), reads=[pg], writes=[s1])
                            S.op("dve", lambda e, s1=s1, pu=pu, fc=fc: e.tensor_tensor(out=h.ap[:, fc, :], in0=s1.ap[:], in1=pu.ap[:, 0:TS], op=ALU.mult), reads=[s1, pu], writes=[h])
                        return h

                    def DN(t, c_, h):
                        o_ = ost.next()
                        for s_ in range(SUB):
                            for hf in range(2):
                                po = pso.next()
                                for fc in range(4):
                                    S.op("pe", lambda e, po=po, fc=fc, s_=s_, hf=hf: e.matmul(po.ap[:], h.ap[:, fc, s_ * 128:(s_ + 1) * 128], c_.ap[:, fc, hf * 512:(hf + 1) * 512], start=(fc == 0), stop=(fc == 3)),
                                         reads=[h, c_], writes=[po], pe_acc=(fc > 0))
                                kev[0] += 1
                                if kev[0] % 2 == 0:
                                    S.op("act", lambda e, po=po, s_=s_, hf=hf: e.activation(out=o_.ap[:, s_, hf * 512:(hf + 1) * 512], in_=po.ap[:], func=AF.Copy, scale=small.ap[:, t, s_, 1:2]),
                                         reads=[po, small], writes=[o_])
                                else:
                                    S.op("dve", lambda e, po=po, s_=s_, hf=hf: e.tensor_scalar(out=o_.ap[:, s_, hf * 512:(hf + 1) * 512], in0=po.ap[:], scalar1=small.ap[:, t, s_, 1:2], scalar2=None, op0=ALU.mult),
                                         reads=[po, small], writes=[o_])
                        S.dma("sp", lambda e, o_=o_, t=t: e.dma_start(out=oslot[t * TS:(t + 1) * TS, :].rearrange("(s p) n -> p s n", p=128), in_=o_.ap[:]), reads=[o_])

                    xq = {0: pre_x(0), 1: pre_x(1)}
                    wq = {0: pre_w(0)}
                    xTq = {0: TT(0, xq.pop(0))}
                    for t in range(NTL):
                        a, b_, c_ = wq.pop(t)
                        if t + 1 < NTL:
                            wq[t + 1] = pre_w(t + 1)
                        if t + 2 < NTL:
                            xq[t + 2] = pre_x(t + 2)
                        h = GU(t, a, b_, xTq.pop(t))
                        if t + 1 < NTL:
                            xTq[t + 1] = TT(t + 1, xq.pop(t + 1))
                        DN(t, c_, h)
                    S.barrier(dummy)
                with contextlib.ExitStack() as ph:
                    bufA = Buf(sb(ph, "F_bA", [128, 8, GT], F32))
                    bufB = Buf(sb(ph, "F_bB", [128, 8, GT], F32))
                    bufC = Buf(sb(ph, "F_bC", [128, 8, GT], F32))
                    x32 = Buf(sb(ph, "F_x32", [128, 8, GT], F32))
                    o1 = Rot([Buf(sb(ph, "F_o1%d" % i, [128, D], F32)) for i in range(8)])
                    o2 = Rot([Buf(sb(ph, "F_o2%d" % i, [128, D], F32)) for i in range(2)])
                    tmp512 = Buf(sb(ph, "F_tmp", [128, GT], F32))
                    rstd = Buf(sb(ph, "F_rstd", [128, GT], F32))
                    psX = Rot([Buf(ps(ph, "F_px%d" % i, [128, 512])) for i in range(4)])
                    psA = Buf(ps(ph, "F_pA", [128, 512]))
                    psB = Buf(ps(ph, "F_pB", [128, 512]))
                    gath = {}

                    def GA(g):
                        lst = []
                        for ii in range(4):
                            i = g * 4 + ii
                            a1, a2 = o1.next(), o2.next()
                            S.dma("pool", lambda e, a1=a1, i=i: e.indirect_dma_start(out=a1.ap[:, :], out_offset=None, in_=oslot[:, :],
                                                                                     in_offset=bass.IndirectOffsetOnAxis(ap=idxAB.ap[:, 0, i:i + 1], axis=0)), reads=[idxAB], writes=[a1])
                            S.dma("pool", lambda e, a2=a2, i=i: e.indirect_dma_start(out=a2.ap[:, :], out_offset=None, in_=oslot[:, :],
                                                                                     in_offset=bass.IndirectOffsetOnAxis(ap=idxAB.ap[:, 1, i:i + 1], axis=0)), reads=[idxAB], writes=[a2])
                            S.op("dve", lambda e, a1=a1, a2=a2: e.tensor_tensor(out=a1.ap[:], in0=a1.ap[:], in1=a2.ap[:], op=ALU.add), reads=[a1, a2], writes=[a1])
                            lst.append(a1)
                        gath[g] = lst

                    def xload(g):
                        S.dma("sp", lambda e, g=g: e.dma_start(out=x32.ap[:], in_=xres[:, :, g * GT:(g + 1) * GT].rearrange("c p n -> p c n")), writes=[x32])

                    def GB(g):
                        for ii in range(4):
                            a1 = gath[g][ii]
                            for hf in range(2):
                                px = psX.next()
                                for cc in range(4):
                                    c = hf * 4 + cc
                                    S.op("pe", lambda e, px=px, cc=cc, c=c, a1=a1: e.transpose(px.ap[:, cc * 128:(cc + 1) * 128], a1.ap[:, c * 128:(c + 1) * 128], ident.ap[:]),
                                         reads=[a1, ident], writes=[px])
                                S.op("dve", lambda e, px=px, hf=hf, ii=ii: e.scalar_tensor_tensor(out=bufB.ap[:, hf * 4:(hf + 1) * 4, ii * 128:(ii + 1) * 128],
                                                                                                  in0=x32.ap[:, hf * 4:(hf + 1) * 4, ii * 128:(ii + 1) * 128], scalar=ALPHA,
                                                                                                  in1=px.ap[:].rearrange("p (c n) -> p c n", n=128), op0=ALU.mult, op1=ALU.add),
                                     reads=[x32, px], writes=[bufB])
                        if g + 1 < NG:
                            xload(g + 1)

                    def LN(g, part):
                        layer_norm(ph, l, 2, g, bufB, bufA, bufC, psA, psB, tmp512, rstd, final=final, part=part)

                    xload(0)
                    GA(0)
                    GB(0)
                    LN(0, 1)
                    for g in range(1, NG):
                        GA(g)
                        GB(g)
                        LN(g - 1, 2)
                        LN(g, 1)
                    LN(NG - 1, 2)
                    S.barrier(dummy)

        S.barrier(dummy)
        for l in range(n_layers):
            with contextlib.ExitStack() as lay:
                Bm_all = [Buf(sb(lay, "Bm%d" % h, [128, 640], F32)) for h in range(8)]
                for h in range(8):
                    S.dma("sp", lambda e, h=h: e.dma_start(out=Bm_all[h].ap[:], in_=relB[l, h, :, :]), writes=[Bm_all[h]])
                    S.op("dve", lambda e, h=h: e.tensor_tensor(out=Bm_all[h].ap[:], in0=Bm_all[h].ap[:], in1=maskM.ap[:], op=ALU.add),
                         reads=[Bm_all[h], maskM], writes=[Bm_all[h]])
                phase_A(l)
                if l == 0:
                    cast_weights(0, "moe")
                if stop_after == ("A", l):
                    break
                phase_B1(l, Bm_all)
            if stop_after == ("B1", l):
                break
            phase_B2(l)
            if stop_after == ("B2", l):
                break
            phase_C(l)
            if stop_after == ("C", l):
                break
            phase_D(l, final=(l == n_layers - 1))
        if debug:
            pass
        S.finish("sp")
        S.emit()
    return nc


def _host_layout(inputs):
    f = lambda a: np.ascontiguousarray(np.asarray(a, dtype=np.float32))
    rel = f(inputs["rel_bias"])
    qi = np.arange(128)[:, None]
    cc = np.arange(640)[None, :]
    idx = np.clip(qi + 512 - cc, -128, 128) + 128
    relB = f(rel[:, :, idx])
    lnp = np.stack([f(inputs["ln1_g"]), f(inputs["ln1_b"]), f(inputs["ln2_g"]), f(inputs["ln2_b"])], axis=1)
    lnp = lnp.reshape(DEPTH, 4, 8, 128).transpose(3, 0, 1, 2).reshape(128, DEPTH * 4 * 8)
    shared = {
        "w_in": f(inputs["w_in"]), "relB": relB, "w_up_a": f(inputs["w_up_a"]), "w_up_b": f(inputs["w_up_b"]),
        "w_out": f(inputs["w_out"]), "lnp": f(lnp), "w_router": f(inputs["w_router"]),
        "brb": f(np.broadcast_to(f(inputs["b_router"])[None, :], (128, NE))),
        "w_gate": f(inputs["w_gate"]), "w_up": f(inputs["w_up"]), "w_down": f(inputs["w_down"]),
    }
    return shared


_NC_CACHE = {}


def kernel(**inputs):
    x = np.asarray(inputs["x"], dtype=np.float32)
    shared = _host_layout(inputs)
    n = x.shape[0]
    in_maps = []
    for b in range(n):
        m = dict(shared)
        m["xT"] = np.ascontiguousarray(x[b].T)
        in_maps.append(m)
    if "nc" not in _NC_CACHE:
        _NC_CACHE["nc"] = build_nc()
    res = run_bass_kernel_spmd(_NC_CACHE["nc"], in_maps, core_ids=list(range(n)))
    out = np.stack([np.ascontiguousarray(r["yT"].T) for r in res.results], axis=0)
    return out.astype(np.float32)
```

```python
import contextlib
import numpy as np
import concourse.bass as bass
import concourse.mybir as mybir
from concourse.bass_utils import run_bass_kernel_spmd

F32 = mybir.dt.float32
BF16 = mybir.dt.bfloat16
I32 = mybir.dt.int32
AF = mybir.ActivationFunctionType
ALU = mybir.AluOpType
AX = mybir.AxisListType

DEPTH = 4
D = 1024
S_LEN = 4096
NG = 8
GT = 512
NE = 16
DF = 512
ALPHA = (2 * DEPTH) ** 0.25
LN_EPS = 1e-5
NEG = -30000.0

ENGS = ["pe", "act", "dve", "pool", "sp"]
NSEM = {"sp": 40, "pool": 24, "act": 8}


class Buf:
    __slots__ = ("ap", "name", "lw", "rd")

    def __init__(self, ap, name=""):
        self.ap = ap
        self.name = name
        self.lw = None
        self.rd = []

    def __getitem__(self, idx):
        return self.ap[idx]


class Sched:
    def __init__(self, nc, stack):
        self.nc = nc
        self.streams = {e: [] for e in ENGS}
        self.count = {e: 0 for e in ENGS}
        self.sem = {e: stack.enter_context(nc.semaphore("prog_" + e)) for e in ENGS}
        self.dsem = {q: [stack.enter_context(nc.semaphore("dma_%s_%d" % (q, i))) for i in range(n)]
                     for q, n in NSEM.items()}
        self.dcount = {q: 0 for q in NSEM}
        self.seen = {e: {} for e in ENGS}
        self.pending = {e: [] for e in ENGS}

    def _deps(self, reads, writes):
        deps = []
        for b in reads:
            if b.lw is not None:
                deps.append(b.lw)
        for b in writes:
            if b.lw is not None:
                deps.append(b.lw)
            deps.extend(b.rd)
        return deps

    def _mark(self, tok, reads, writes):
        for b in reads:
            b.rd.append(tok)
            if len(b.rd) > 48:
                b.rd = b.rd[-48:]
        for b in writes:
            b.lw = tok
            b.rd = []

    def _waits(self, eng, deps, skip_same_pe=False):
        if self.pending[eng]:
            deps = list(deps) + self.pending[eng]
            self.pending[eng] = []
        need = {}
        for (kind, a, b) in deps:
            if kind == "c":
                if skip_same_pe and a == "pe" and eng == "pe":
                    continue
                key = ("c", a)
            else:
                key = ("d", a)
            if self.seen[eng].get(key, 0) >= b:
                continue
            if need.get(key, 0) < b:
                need[key] = b
        out = []
        for key, val in need.items():
            self.seen[eng][key] = val
            if key[0] == "c":
                sem = self.sem[key[1]]
            else:
                sem = self.dsem[key[1][0]][key[1][1]]
            out.append((sem, val))
        return out

    def op(self, eng, fn, reads=(), writes=(), pe_acc=False):
        deps = self._deps(reads, writes)
        waits = self._waits(eng, deps, skip_same_pe=pe_acc)
        self.count[eng] += 1
        tok = ("c", eng, self.count[eng])
        self.streams[eng].append((_freeze(fn), waits, (self.sem[eng], 1)))
        self._mark(tok, reads, writes)
        return tok

    def dma(self, q, fn, reads=(), writes=()):
        deps = self._deps(reads, writes)
        n = NSEM[q]
        slot = self.dcount[q] % n
        gen = self.dcount[q] // n
        self.dcount[q] += 1
        if gen > 0:
            deps.append(("d", (q, slot), 16 * gen))
        waits = self._waits(q, deps)
        tok = ("d", (q, slot), 16 * (gen + 1))
        self.streams[q].append((_freeze(fn), waits, (self.dsem[q][slot], 16)))
        self._mark(tok, reads, writes)
        return tok

    def all_tokens(self):
        toks = [("c", e, self.count[e]) for e in ENGS if self.count[e] > 0]
        for q, n in NSEM.items():
            for slot in range(min(n, self.dcount[q])):
                last = ((self.dcount[q] - 1 - slot) // n)
                toks.append(("d", (q, slot), 16 * (last + 1)))
        return toks

    def barrier(self, dummy):
        toks = self.all_tokens()
        self.pending["pool"].extend(toks)
        tb = self.op("pool", lambda e: e.memset(dummy.ap[:], 0.0), writes=[dummy])
        for e in ENGS:
            if e != "pool":
                self.pending[e].append(tb)

    def finish(self, eng="sp"):
        waits = self._waits(eng, self.all_tokens())
        self.streams[eng].append((None, waits, None))

    def emit(self):
        nc = self.nc
        streams = self.streams
        with nc.Block() as block:
            def run(engine, items):
                for fn, waits, inc in items:
                    for sem, val in waits:
                        engine.wait_ge(sem, val)
                    if fn is not None:
                        name, a, k = fn
                        getattr(engine, name)(*a, **k).then_inc(inc[0], inc[1])

            @block.tensor
            def _(e):
                run(e, streams["pe"])

            @block.scalar
            def _(e):
                run(e, streams["act"])

            @block.vector
            def _(e):
                run(e, streams["dve"])

            @block.gpsimd
            def _(e):
                run(e, streams["pool"])

            @block.sync
            def _(e):
                run(e, streams["sp"])


class _Rec:
    def __init__(self):
        self.call = None

    def __getattr__(self, name):
        def f(*a, **k):
            self.call = (name, a, k)
            return self
        return f


def _freeze(fn):
    r = _Rec()
    fn(r)
    assert r.call is not None
    return r.call


class Rot:
    def __init__(self, bufs):
        self.bufs = bufs
        self.i = 0

    def next(self):
        b = self.bufs[self.i % len(self.bufs)]
        self.i += 1
        return b


def build_nc(n_layers=DEPTH, stop_after=None, debug=False):
    nc = bass.Bass("TRN2", target_bir_lowering=False)
    dt_in = lambda name, shape: nc.dram_tensor(name, list(shape), F32, kind="ExternalInput").ap()
    xT_in = dt_in("xT", [D, S_LEN])
    w_in = dt_in("w_in", [DEPTH, D, 5120])
    relB = dt_in("relB", [DEPTH, 8, 128, 640])
    w_up_a = dt_in("w_up_a", [DEPTH, 512, D])
    w_up_b = dt_in("w_up_b", [DEPTH, 512, D])
    w_out = dt_in("w_out", [DEPTH, D, D])
    lnp = dt_in("lnp", [128, DEPTH * 4 * 8])
    w_router = dt_in("w_router", [D, NE])
    brb = dt_in("brb", [128, NE])
    w_gate = dt_in("w_gate", [DEPTH, NE, D, DF])
    w_up = dt_in("w_up", [DEPTH, NE, D, DF])
    w_down = dt_in("w_down", [DEPTH, NE, DF, D])
    yT_out = nc.dram_tensor("yT", [D, S_LEN], F32, kind="ExternalOutput").ap()

    def scratch(name, shape, dt):
        kind = "ExternalOutput" if (debug and name in debug) else "Internal"
        return nc.dram_tensor(name, list(shape), dt, kind=kind).ap()

    wb_in = [scratch("wb_in%d" % l, [D, 5120], BF16) for l in range(n_layers)]
    wb_upa = [scratch("wb_upa%d" % l, [512, D], BF16) for l in range(n_layers)]
    wb_upb = [scratch("wb_upb%d" % l, [512, D], BF16) for l in range(n_layers)]
    wb_out = [scratch("wb_out%d" % l, [D, D], BF16) for l in range(n_layers)]
    wb_g = [scratch("wb_g%d" % l, [NE * 128, 8 * DF], BF16) for l in range(n_layers)]
    wb_u = [scratch("wb_u%d" % l, [NE * 128, 8 * DF], BF16) for l in range(n_layers)]
    wb_d = [scratch("wb_d%d" % l, [NE * 128, 4 * D], BF16) for l in range(n_layers)]
    TS = 256
    SUB = TS // 128
    NTL = 32 + NE
    NSLOT = NTL * TS
    x1tok = scratch("x1tok", [S_LEN, D], BF16)
    slotmap = scratch("slotmap", [NSLOT, 2], F32)
    oslot = scratch("oslot", [NSLOT, D], F32)
    xres = scratch("xres", [8, 128, S_LEN], F32)
    qkT = scratch("qkT", [16, 128, S_LEN], BF16)
    Vd = scratch("Vd", [S_LEN, 1024], BF16)
    YTd = scratch("YTd", [8, 128, S_LEN], BF16)
    gTd = scratch("gTd", [16, 128, S_LEN], BF16)
    dbg = {}
    if debug:
        for nm in debug:
            pass

    with contextlib.ExitStack() as st:
        S = Sched(nc, st)

        uid = [0]

        def sb(stack, name, shape, dt):
            uid[0] += 1
            return stack.enter_context(nc.sbuf_tensor("%s_%d" % (name, uid[0]), list(shape), dt))

        def ps(stack, name, shape, dt=F32):
            uid[0] += 1
            return stack.enter_context(nc.psum_tensor("%s_%d" % (name, uid[0]), list(shape), dt))

        xbf_t = sb(st, "xbf", [128, 8, S_LEN], BF16)
        xbf = [Buf(xbf_t[:, :, g * GT:(g + 1) * GT], "xbf%d" % g) for g in range(NG)]
        dummy = Buf(sb(st, "bar_dummy", [128, 8], F32))
        lnp_sb = Buf(sb(st, "lnp_sb", [128, DEPTH * 4 * 8], F32))
        ones_d = Buf(sb(st, "ones_d", [128, 128], F32))
        ident = Buf(sb(st, "ident", [128, 128], F32))
        identb = Buf(sb(st, "identb", [128, 128], BF16))
        trimask = Buf(sb(st, "trimask", [128, 128], BF16))
        ntri = Buf(sb(st, "ntri", [128, 128], BF16))
        nstr = Buf(sb(st, "nstr", [128, 128], BF16))
        wr32 = Buf(sb(st, "wr32", [128, 8, NE], F32))
        brb_sb = Buf(sb(st, "brb_sb", [128, NE], F32))
        pstr = Buf(sb(st, "pstr", [128, 128], BF16))
        pones = Buf(sb(st, "pones", [128, 128], BF16))
        pidx = Buf(sb(st, "pidx", [128, 1], F32))
        tokid = Buf(sb(st, "tokid", [128, 32], F32))
        tvals = Buf(sb(st, "tvals", [128, 48, NE], F32))
        zeros = Buf(sb(st, "zeros", [128, 256], F32))
        maskM = Buf(sb(st, "maskM", [128, 640], F32))
        lg_glob = Buf(sb(st, "lg_glob", [128, 32, NE], F32))
        itmp = Buf(sb(st, "itmp", [128, 48 * NE], I32))

        S.dma("sp", lambda e: e.dma_start(out=lnp_sb.ap[:], in_=lnp[:, :]), writes=[lnp_sb])
        S.dma("sp", lambda e: e.dma_start(out=brb_sb.ap[:], in_=brb[:, :]), writes=[brb_sb])
        S.dma("sp", lambda e: e.dma_start(out=wr32.ap[:], in_=w_router.rearrange("(c p) n -> p c n", p=128)), writes=[wr32])
        S.op("pool", lambda e: e.memset(ones_d.ap[:], 1.0 / D), writes=[ones_d])
        for t_, dt_ in ((ident, F32), (identb, BF16)):
            S.op("pool", lambda e, t_=t_: e.memset(t_.ap[:], 0.0), writes=[t_])
            S.op("pool", lambda e, t_=t_: e.affine_select(out=t_.ap[:], in_=t_.ap[:], pattern=[[-1, 128]], compare_op=ALU.not_equal,
                                                           fill=1.0, base=0, channel_multiplier=1), reads=[t_], writes=[t_])
        S.op("pool", lambda e: e.memset(trimask.ap[:], 1.0), writes=[trimask])
        S.op("pool", lambda e: e.affine_select(out=trimask.ap[:], in_=trimask.ap[:], pattern=[[1, 128]], compare_op=ALU.is_gt,
                                               fill=0.0, base=0, channel_multiplier=-1), reads=[trimask], writes=[trimask])
        S.op("pool", lambda e: e.memset(ntri.ap[:], -1.0), writes=[ntri])
        S.op("pool", lambda e: e.affine_select(out=ntri.ap[:], in_=ntri.ap[:], pattern=[[-1, 128]], compare_op=ALU.is_ge,
                                               fill=0.0, base=0, channel_multiplier=1), reads=[ntri], writes=[ntri])
        S.op("pool", lambda e: e.memset(nstr.ap[:], -1.0), writes=[nstr])
        S.op("pool", lambda e: e.affine_select(out=nstr.ap[:], in_=nstr.ap[:], pattern=[[1, 128]], compare_op=ALU.is_gt,
                                               fill=0.0, base=0, channel_multiplier=-1), reads=[nstr], writes=[nstr])
        S.op("pool", lambda e: e.memset(maskM.ap[:], 0.0), writes=[maskM])
        S.op("pool", lambda e: e.affine_select(out=maskM.ap[0:64, :], in_=maskM.ap[0:64, :], pattern=[[-1, 640]], compare_op=ALU.is_ge,
                                               fill=NEG, base=575, channel_multiplier=0), reads=[maskM], writes=[maskM])
        S.op("pool", lambda e: e.affine_select(out=maskM.ap[64:128, :], in_=maskM.ap[64:128, :], pattern=[[1, 640]], compare_op=ALU.is_ge,
                                               fill=NEG, base=-64, channel_multiplier=0), reads=[maskM], writes=[maskM])
        S.op("pool", lambda e: e.memset(pstr.ap[:], 1.0), writes=[pstr])
        S.op("pool", lambda e: e.affine_select(out=pstr.ap[:], in_=pstr.ap[:], pattern=[[1, 128]], compare_op=ALU.is_gt,
                                               fill=0.0, base=0, channel_multiplier=-1), reads=[pstr], writes=[pstr])
        S.op("pool", lambda e: e.memset(pones.ap[:], 1.0), writes=[pones])
        S.op("pool", lambda e: e.memset(zeros.ap[:], 0.0), writes=[zeros])
        S.op("pool", lambda e: e.iota(itmp.ap[:, 0:1], pattern=[[0, 1]], base=0, channel_multiplier=1), writes=[itmp])
        S.op("dve", lambda e: e.tensor_copy(out=pidx.ap[:], in_=itmp.ap[:, 0:1]), reads=[itmp], writes=[pidx])
        S.op("pool", lambda e: e.iota(itmp.ap[:, 0:32], pattern=[[128, 32]], base=0, channel_multiplier=1), reads=[pidx], writes=[itmp])
        S.op("dve", lambda e: e.tensor_copy(out=tokid.ap[:], in_=itmp.ap[:, 0:32]), reads=[itmp], writes=[tokid])
        S.op("pool", lambda e: e.iota(itmp.ap[:], pattern=[[1, 48], [0, NE]], base=0, channel_multiplier=0), reads=[tokid], writes=[itmp])
        S.op("dve", lambda e: e.tensor_copy(out=tvals.ap[:], in_=itmp.ap[:].rearrange("p (t e) -> p t e", e=NE)), reads=[itmp], writes=[tvals])

        def flat2(ap, n):
            names = " ".join("d%d" % i for i in range(ap.ndim))
            flat = ap.rearrange("%s -> (%s)" % (names, names))
            return flat.rearrange("(r c) -> r c", c=1024)

        def cast_weights(l, which):
            items = []
            if which == "mix":
                items = [(wb_in[l], w_in[l]), (wb_upa[l], w_up_a[l]), (wb_upb[l], w_up_b[l]), (wb_out[l], w_out[l])]
            else:
                for e_ in range(NE):
                    for dst, src, a in ((wb_g[l], w_gate[l], 8), (wb_u[l], w_up[l], 8), (wb_d[l], w_down[l], 4)):
                        S.dma("pool", lambda e, dst=dst, src=src, a=a, e_=e_: e.dma_start(
                            out=dst[e_ * 128:(e_ + 1) * 128, :].rearrange("p (c n) -> p c n", c=a),
                            in_=src[e_].rearrange("(c p) n -> p c n", p=128)))
                return
            for dst, src in items:
                n = 1
                for s_ in dst.shape:
                    n *= s_
                d2 = flat2(dst, n)
                s2 = flat2(src, n)
                R = n // 1024
                step = 1024
                for r0 in range(0, R, step):
                    r1 = min(R, r0 + step)
                    S.dma("pool", lambda e, d2=d2, s2=s2, r0=r0, r1=r1: e.dma_start(out=d2[r0:r1, :], in_=s2[r0:r1, :]))

        def cast_gen(l):
            for dst, src in [(wb_in[l], w_in[l]), (wb_upa[l], w_up_a[l]), (wb_upb[l], w_up_b[l]), (wb_out[l], w_out[l])]:
                n = 1
                for s_ in dst.shape:
                    n *= s_
                d2 = flat2(dst, n)
                s2 = flat2(src, n)
                R = n // 1024
                for r0 in range(0, R, 1024):
                    r1 = min(R, r0 + 1024)
                    S.dma("pool", lambda e, d2=d2, s2=s2, r0=r0, r1=r1: e.dma_start(out=d2[r0:r1, :], in_=s2[r0:r1, :]))
                    yield
            for e_ in range(NE):
                for dst, src, a in ((wb_g[l], w_gate[l], 8), (wb_u[l], w_up[l], 8), (wb_d[l], w_down[l], 4)):
                    S.dma("pool", lambda e, dst=dst, src=src, a=a, e_=e_: e.dma_start(
                        out=dst[e_ * 128:(e_ + 1) * 128, :].rearrange("p (c n) -> p c n", c=a),
                        in_=src[e_].rearrange("(c p) n -> p c n", p=128)))
                    yield

        cast_weights(0, "mix")
        for g in range(NG):
            S.dma("pool", lambda e, g=g: e.dma_start(out=xbf[g].ap, in_=xT_in[:, g * GT:(g + 1) * GT].rearrange("(c p) n -> p c n", p=128)),
                  writes=[xbf[g]])

        def lncol(l, which, c):
            i = (l * 4 + which) * 8 + c
            return lnp_sb.ap[:, i:i + 1]

        def layer_norm(ph, l, which, g, y, tbuf, obuf, psA, psB, tmp512, rstd, final, part=0):
            if part in (0, 1):
                ln_part1(y, tbuf, obuf, psA)
            if part in (0, 2):
                ln_part2(l, which, g, tbuf, obuf, psB, tmp512, rstd, final)

        def ln_part1(y, tbuf, obuf, psA):
            for c in range(8):
                S.op("pe", lambda e, c=c: e.matmul(psA.ap[:], ones_d.ap[:], y.ap[:, c, :], start=(c == 0), stop=(c == 7)),
                     reads=[ones_d, y], writes=[psA], pe_acc=(c > 0))
            for c in range(8):
                S.op("dve", lambda e, c=c: e.tensor_tensor(out=tbuf.ap[:, c, :], in0=y.ap[:, c, :], in1=psA.ap[:], op=ALU.subtract),
                     reads=[y, psA], writes=[tbuf])
            S.op("act", lambda e: e.activation(out=obuf.ap[:], in_=tbuf.ap[:], func=AF.Square), reads=[tbuf], writes=[obuf])

        def ln_part2(l, which, g, tbuf, obuf, psB, tmp512, rstd, final):
            for c in range(8):
                S.op("pe", lambda e, c=c: e.matmul(psB.ap[:], ones_d.ap[:], obuf.ap[:, c, :], start=(c == 0), stop=(c == 7)),
                     reads=[ones_d, obuf], writes=[psB], pe_acc=(c > 0))
            S.op("act", lambda e: e.activation(out=tmp512.ap[:], in_=psB.ap[:], func=AF.Ln, bias=LN_EPS), reads=[psB], writes=[tmp512])
            S.op("act", lambda e: e.activation(out=rstd.ap[:], in_=tmp512.ap[:], func=AF.Exp, scale=-0.5), reads=[tmp512], writes=[rstd])
            for c in range(8):
                S.op("dve", lambda e, c=c: e.tensor_tensor(out=tbuf.ap[:, c, :], in0=tbuf.ap[:, c, :], in1=rstd.ap[:], op=ALU.mult),
                     reads=[tbuf, rstd], writes=[tbuf])
            for c in range(8):
                S.op("act", lambda e, c=c: e.activation(out=obuf.ap[:, c, :], in_=tbuf.ap[:, c, :], func=AF.Identity,
                                                        scale=lncol(l, which, c), bias=lncol(l, which + 1, c)),
                     reads=[tbuf, lnp_sb], writes=[obuf])
            for c in range(8):
                S.op("act", lambda e, c=c: e.activation(out=xbf[g].ap[:, c, :], in_=tbuf.ap[:, c, :], func=AF.Identity,
                                                        scale=lncol(l, which, c), bias=lncol(l, which + 1, c)),
                     reads=[tbuf, lnp_sb], writes=[xbf[g]])
            if final:
                dst = yT_out[:, g * GT:(g + 1) * GT].rearrange("(c p) n -> p c n", p=128)
            else:
                dst = xres[:, :, g * GT:(g + 1) * GT].rearrange("c p n -> p c n")
            S.dma("sp", lambda e: e.dma_start(out=dst, in_=obuf.ap[:]), reads=[obuf])

        def phase_A(l):
            with contextlib.ExitStack() as ph:
                wt = Rot([Buf(sb(ph, "A_w%d" % i, [128, 8, 512], BF16)) for i in range(2)])
                stg = Rot([Buf(sb(ph, "A_s%d" % i, [128, 4, 512], BF16)) for i in range(3)])
                pss = Rot([Buf(ps(ph, "A_p%d" % i, [128, 512])) for i in range(6)])
                k_ev = [0]

                def evac(dst_ap, dst_buf, p, func, scale):
                    k_ev[0] += 1
                    if func is not None:
                        S.op("act", lambda e: e.activation(out=dst_ap, in_=p.ap[:], func=func), reads=[p], writes=[dst_buf])
                    elif k_ev[0] % 2 == 0:
                        S.op("act", lambda e: e.activation(out=dst_ap, in_=p.ap[:], func=AF.Copy, scale=scale), reads=[p], writes=[dst_buf])
                    else:
                        S.op("dve", lambda e: e.tensor_scalar(out=dst_ap, in0=p.ap[:], scalar1=scale, scalar2=None, op0=ALU.mult),
                             reads=[p], writes=[dst_buf])

                for cb in range(6):
                    w = wt.next()
                    S.dma("sp", lambda e, w=w, cb=cb: e.dma_start(out=w.ap[:], in_=wb_in[l][:, cb * 512:(cb + 1) * 512].rearrange("(c p) n -> p c n", p=128)),
                          writes=[w])
                    if cb in (2, 5):
                        vcol = 0 if cb == 2 else 512
                        for tt in range(8):
                            s_ = stg.next()
                            for i in range(4):
                                p = pss.next()
                                for kc in range(8):
                                    S.op("pe", lambda e, p=p, w=w, kc=kc, tt=tt, i=i: e.matmul(p.ap[:], xbf[tt].ap[:, kc, i * 128:(i + 1) * 128], w.ap[:, kc, :],
                                                                                               start=(kc == 0), stop=(kc == 7)),
                                         reads=[xbf[tt], w], writes=[p], pe_acc=(kc > 0))
                                evac(s_.ap[:, i, :], s_, p, None, 1.0)
                            S.dma("sp", lambda e, s_=s_, tt=tt, vcol=vcol: e.dma_start(
                                out=Vd[tt * 512:(tt + 1) * 512, vcol:vcol + 512].rearrange("(i p) n -> p i n", p=128), in_=s_.ap[:]), reads=[s_])
                    else:
                        if cb >= 6:
                            dst_t, cbase, func, scale = gTd, (cb - 6) * 4, AF.Sigmoid, 1.0
                        else:
                            dst_t, cbase, func = qkT, {0: 0, 1: 4, 3: 8, 4: 12}[cb], None
                            scale = 0.125 if cb in (0, 3) else 1.0
                        for g in range(NG):
                            s_ = stg.next()
                            for c in range(4):
                                p = pss.next()
                                for kc in range(8):
                                    S.op("pe", lambda e, p=p, w=w, kc=kc, g=g, c=c: e.matmul(p.ap[:], w.ap[:, kc, c * 128:(c + 1) * 128], xbf[g].ap[:, kc, :],
                                                                                             start=(kc == 0), stop=(kc == 7)),
                                         reads=[xbf[g], w], writes=[p], pe_acc=(kc > 0))
                                evac(s_.ap[:, c, :], s_, p, func, scale)
                            S.dma("sp", lambda e, s_=s_, g=g, dst_t=dst_t, cbase=cbase: e.dma_start(
                                out=dst_t[cbase:cbase + 4, :, g * GT:(g + 1) * GT].rearrange("c p n -> p c n"), in_=s_.ap[:]), reads=[s_])
                S.barrier(dummy)

        def phase_B1(l, Bm_all):
            with contextlib.ExitStack() as ph:
                qT = Rot([Buf(sb(ph, "B_q%d" % i, [128, S_LEN], BF16)) for i in range(2)])
                kT = Rot([Buf(sb(ph, "B_k%d" % i, [128, S_LEN], BF16)) for i in range(2)])
                Vt = Rot([Buf(sb(ph, "B_v%d" % i, [128, 32, 128], BF16)) for i in range(2)])
                YTs = Rot([Buf(sb(ph, "B_y%d" % i, [128, S_LEN], BF16)) for i in range(2)])
                Sb = Rot([Buf(sb(ph, "B_sb%d" % i, [128, 640], F32)) for i in range(4)])
                Pe = Rot([Buf(sb(ph, "B_pe%d" % i, [128, 640], BF16)) for i in range(8)])
                Pn = Rot([Buf(sb(ph, "B_pn%d" % i, [128, 640], BF16)) for i in range(4)])
                PnT = Rot([Buf(sb(ph, "B_pt%d" % i, [128, 5, 128], BF16)) for i in range(8)])
                stt = Rot([Buf(sb(ph, "B_st%d" % i, [128, 4], F32)) for i in range(12)])
                psS = Rot([Buf(ps(ph, "B_ps%d" % i, [128, 1024])) for i in range(2)])
                psT = Rot([Buf(ps(ph, "B_pT%d" % i, [128, 1024], BF16)) for i in range(2)])
                psY = Rot([Buf(ps(ph, "B_pY%d" % i, [128, 512])) for i in range(2)])

                for hp in range(4):
                    def qkv_load(hp_):
                        q2, k2, v2 = qT.next(), kT.next(), Vt.next()
                        S.dma("sp", lambda e: e.dma_start(out=q2.ap[:], in_=qkT[hp_, :, :]), writes=[q2])
                        S.dma("sp", lambda e: e.dma_start(out=k2.ap[:], in_=qkT[4 + hp_, :, :]), writes=[k2])
                        S.dma("sp", lambda e: e.dma_start(out=v2.ap[:], in_=Vd[:, hp_ * 128:(hp_ + 1) * 128].rearrange("(t p) c -> p t c", p=128)), writes=[v2])
                        return q2, k2, v2
                    if hp == 0:
                        nxt_qkv = qkv_load(0)
                    q_, k_, v_ = nxt_qkv
                    y_ = YTs.next()
                    if hp + 1 < 4:
                        nxt_qkv = qkv_load(hp + 1)
                    bms = [Bm_all[hp * 2], Bm_all[hp * 2 + 1]]

                    state = {}

                    def s1a(b):
                        nk = min(b + 1, 5)
                        W = 128 * nk
                        k0 = (b - nk + 1) * 128
                        w1 = min(W, 512)
                        hs = []
                        for hh in range(2):
                            r0, r1 = hh * 64, hh * 64 + 64
                            pS = psS.next()
                            S.op("pe", lambda e, pS=pS, r0=r0, r1=r1: e.matmul(pS.ap[:, 0:w1], q_.ap[r0:r1, b * 128:(b + 1) * 128], k_.ap[r0:r1, k0:k0 + w1], start=True, stop=True),
                                 reads=[q_, k_], writes=[pS])
                            if W > 512:
                                S.op("pe", lambda e, pS=pS, r0=r0, r1=r1: e.matmul(pS.ap[:, 512:640], q_.ap[r0:r1, b * 128:(b + 1) * 128], k_.ap[r0:r1, k0 + 512:k0 + 640], start=True, stop=True),
                                     reads=[q_, k_], writes=[pS])
                            hs.append(dict(pS=pS, sbf=Sb.next(), pe_=Pe.next(), st_=stt.next()))
                        state[b] = dict(nk=nk, W=W, c0=640 - W, hs=hs)

                    def s1b(b):
                        s_ = state[b]
                        W, c0 = s_["W"], s_["c0"]
                        for hh in range(2):
                            d = s_["hs"][hh]
                            S.op("dve", lambda e, d=d, hh=hh: e.tensor_tensor(out=d["sbf"].ap[:, 0:W], in0=d["pS"].ap[:, 0:W], in1=bms[hh].ap[:, c0:640], op=ALU.add),
                                 reads=[d["pS"], bms[hh]], writes=[d["sbf"]])
                        for hh in range(2):
                            d = s_["hs"][hh]
                            S.op("dve", lambda e, d=d: e.tensor_reduce(out=d["st_"].ap[:, 0:1], in_=d["sbf"].ap[:, 0:W], axis=AX.X, op=ALU.max, negate=True),
                                 reads=[d["sbf"]], writes=[d["st_"]])

                    def s1c(b):
                        s_ = state[b]
                        W = s_["W"]
                        for hh in range(2):
                            d = s_["hs"][hh]
                            S.op("act", lambda e, d=d: e.activation(out=d["pe_"].ap[:, 0:W], in_=d["sbf"].ap[:, 0:W], func=AF.Exp, bias=d["st_"].ap[:, 0:1], accum_out=d["st_"].ap[:, 1:2]),
                                 reads=[d["sbf"], d["st_"]], writes=[d["pe_"], d["st_"]])

                    def s2a(b):
                        s_ = state[b]
                        W = s_["W"]
                        for hh in range(2):
                            d = s_["hs"][hh]
                            S.op("dve", lambda e, d=d: e.reciprocal(out=d["st_"].ap[:, 2:3], in_=d["st_"].ap[:, 1:2]), reads=[d["st_"]], writes=[d["st_"]])
                        for hh in range(2):
                            d = s_["hs"][hh]
                            d["pn"] = Pn.next()
                            S.op("dve", lambda e, d=d: e.tensor_scalar(out=d["pn"].ap[:, 0:W], in0=d["pe_"].ap[:, 0:W], scalar1=d["st_"].ap[:, 2:3], scalar2=None, op0=ALU.mult),
                                 reads=[d["pe_"], d["st_"]], writes=[d["pn"]])

                    def s2b(b):
                        s_ = state[b]
                        nk = s_["nk"]
                        for hh in range(2):
                            d = s_["hs"][hh]
                            d["pT"] = psT.next()
                            for i in range(nk):
                                S.op("pe", lambda e, d=d, i=i: e.transpose(d["pT"].ap[:, i * 128:(i + 1) * 128], d["pn"].ap[:, i * 128:(i + 1) * 128], identb.ap[:]),
                                     reads=[d["pn"], identb], writes=[d["pT"]])

                    def s2c(b):
                        s_ = state[b]
                        W, nk = s_["W"], s_["nk"]
                        for hh in range(2):
                            d = s_["hs"][hh]
                            d["pnT"] = PnT.next()
                            S.op("act", lambda e, d=d: e.activation(out=d["pnT"].ap[:, 0:nk, :], in_=d["pT"].ap[:, 0:W].rearrange("p (i n) -> p i n", n=128), func=AF.Copy),
                                 reads=[d["pT"]], writes=[d["pnT"]])

                    def s3(b):
                        s_ = state.pop(b)
                        nk, hs = s_["nk"], s_["hs"]
                        j0 = b - nk + 1
                        pY = psY.next()
                        for hh in range(2):
                            d = hs[hh]
                            for i in range(nk):
                                S.op("pe", lambda e, d=d, i=i, hh=hh: e.matmul(pY.ap[hh * 64:(hh + 1) * 64, 0:128], v_.ap[:, j0 + i, hh * 64:(hh + 1) * 64], d["pnT"].ap[:, i, :],
                                                                               start=(i == 0), stop=(i == nk - 1)),
                                     reads=[v_, d["pnT"]], writes=[pY], pe_acc=(i > 0))
                        S.op("act", lambda e: e.activation(out=y_.ap[:, b * 128:(b + 1) * 128], in_=pY.ap[:, 0:128], func=AF.Copy), reads=[pY], writes=[y_])

                    U = 32
                    for s in range(U + 4):
                        if s < U:
                            s1a(s)
                        if 0 <= s - 2 < U:
                            s2a(s - 2)
                            s2b(s - 2)
                        if s < U:
                            s1b(s)
                        if 0 <= s - 2 < U:
                            s2c(s - 2)
                        if s < U:
                            s1c(s)
                        if 0 <= s - 4 < U:
                            s3(s - 4)
                    S.dma("sp", lambda e, y_=y_, hp=hp: e.dma_start(out=YTd[hp, :, :], in_=y_.ap[:]), reads=[y_])
                S.barrier(dummy)

        def phase_B2(l):
            with contextlib.ExitStack() as ph:
                qT = Rot([Buf(sb(ph, "C_q%d" % i, [128, S_LEN], BF16)) for i in range(2)])
                kT = Rot([Buf(sb(ph, "C_k%d" % i, [128, S_LEN], BF16)) for i in range(2)])
                Vt = Rot([Buf(sb(ph, "C_v%d" % i, [128, 32, 128], BF16)) for i in range(2)])
                YTs = Rot([Buf(sb(ph, "C_y%d" % i, [128, S_LEN], BF16)) for i in range(2)])
                NB = 10
                Eb = Rot([Buf(sb(ph, "C_e%d" % i, [128, 512], BF16)) for i in range(NB)])
                SPb = Rot([Buf(sb(ph, "C_s%d" % i, [128, 512], BF16)) for i in range(NB)])
                Xb = Rot([Buf(sb(ph, "C_x%d" % i, [128, 512], BF16)) for i in range(NB)])
                Ab = Rot([Buf(sb(ph, "C_a%d" % i, [128, 512], BF16)) for i in range(NB)])
                psZ = Rot([Buf(ps(ph, "C_pz%d" % i, [128, 512])) for i in range(3)])
                psP = [Buf(ps(ph, "C_pp%d" % i, [128, 512])) for i in range(2)]
                psY = Buf(ps(ph, "C_pY", [128, 512]))
                gw = Buf(sb(ph, "C_gw", [128, 8, 512], BF16))
                gst = Rot([Buf(sb(ph, "C_gs%d" % i, [128, 4, 512], BF16)) for i in range(2)])
                gps = Rot([Buf(ps(ph, "C_gp%d" % i, [128, 512])) for i in range(2)])

                def gate_units():
                    for cb in range(6, 10):
                        S.dma("sp", lambda e, cb=cb: e.dma_start(out=gw.ap[:], in_=wb_in[l][:, cb * 512:(cb + 1) * 512].rearrange("(c p) n -> p c n", p=128)), writes=[gw])
                        for g in range(NG):
                            st_ = gst.next()
                            for c in range(4):
                                p = gps.next()
                                for kc in range(8):
                                    S.op("pe", lambda e, p=p, kc=kc, g=g, c=c: e.matmul(p.ap[:], gw.ap[:, kc, c * 128:(c + 1) * 128], xbf[g].ap[:, kc, :], start=(kc == 0), stop=(kc == 7)),
                                         reads=[xbf[g], gw], writes=[p], pe_acc=(kc > 0))
                                    if kc % 2 == 1 and kc < 7:
                                        yield
                                S.op("dve", lambda e, p=p, st_=st_, c=c: e.tensor_copy(out=st_.ap[:, c, :], in_=p.ap[:]), reads=[p], writes=[st_])
                                if c == 3:
                                    S.dma("sp", lambda e, st_=st_, g=g, cb=cb: e.dma_start(
                                        out=gTd[(cb - 6) * 4:(cb - 6) * 4 + 4, :, g * GT:(g + 1) * GT].rearrange("c p n -> p c n"), in_=st_.ap[:]), reads=[st_])
                                yield
                gate_gen = gate_units()
                cgen = cast_gen(l + 1) if l + 1 < n_layers else iter(())

                for hp in range(4):
                    def qkv_load(hp_):
                        q2, k2, v2 = qT.next(), kT.next(), Vt.next()
                        S.dma("sp", lambda e: e.dma_start(out=q2.ap[:], in_=qkT[8 + hp_, :, :]), writes=[q2])
                        S.dma("sp", lambda e: e.dma_start(out=k2.ap[:], in_=qkT[12 + hp_, :, :]), writes=[k2])
                        S.dma("sp", lambda e: e.dma_start(out=v2.ap[:], in_=Vd[:, 512 + hp_ * 128:512 + (hp_ + 1) * 128].rearrange("(t p) c -> p t c", p=128)), writes=[v2])
                        return q2, k2, v2
                    if hp == 0:
                        nxt_qkv = qkv_load(0)
                    q_, k_, v_ = nxt_qkv
                    y_ = YTs.next()
                    if hp + 1 < 4:
                        nxt_qkv = qkv_load(hp + 1)
                    steps = [(G, j) for G in range(8) for j in range(4 * G + 3, -1, -1)]
                    state = {}
                    prev_sp = {0: None, 1: None}

                    def stage1(u):
                        G, j = steps[u]
                        o = j - 4 * G
                        qs = max(o, 0) * 128
                        st_u = []
                        for hh in range(2):
                            r0, r1 = hh * 64, hh * 64 + 64
                            pz, eb, sp = psZ.next(), Eb.next(), SPb.next()
                            S.op("pe", lambda e, pz=pz, r0=r0, r1=r1: e.matmul(pz.ap[:, qs:512], k_.ap[r0:r1, j * 128:(j + 1) * 128], q_.ap[r0:r1, G * 512 + qs:(G + 1) * 512],
                                                                               start=True, stop=True), reads=[q_, k_], writes=[pz])
                            st_u.append(dict(pz=pz, eb=eb, sp=sp))
                        for hh in range(2):
                            d = st_u[hh]
                            S.op("act", lambda e, d=d: e.activation(out=d["eb"].ap[:, qs:512], in_=d["pz"].ap[:, qs:512], func=AF.Exp), reads=[d["pz"]], writes=[d["eb"]])
                        for hh in range(2):
                            d = st_u[hh]
                            S.op("act", lambda e, d=d: e.activation(out=d["sp"].ap[:, qs:512], in_=d["eb"].ap[:, qs:512], func=AF.Ln, bias=1.0), reads=[d["eb"]], writes=[d["sp"]])
                        if o >= 0:
                            for hh in range(2):
                                d = st_u[hh]
                                S.op("pool", lambda e, d=d: e.tensor_tensor(out=d["sp"].ap[:, qs:qs + 128], in0=d["sp"].ap[:, qs:qs + 128], in1=trimask.ap[:], op=ALU.mult),
                                     reads=[d["sp"], trimask], writes=[d["sp"]])
                        state[u] = dict(G=G, j=j, o=o, qs=qs, hs=st_u)

                    def stage2(u):
                        s_ = state[u]
                        G, j, o, qs = s_["G"], s_["j"], s_["o"], s_["qs"]
                        first = (j == 4 * G + 3)
                        for hh in range(2):
                            d = s_["hs"][hh]
                            if not first:
                                psp, pqs = prev_sp[hh]
                                S.op("pe", lambda e, hh=hh, psp=psp, pqs=pqs: e.matmul(psP[hh].ap[:, pqs:512], nstr.ap[:], psp.ap[:, pqs:512], start=False, stop=False, skip_group_check=True),
                                     reads=[nstr, psp], writes=[psP[hh]], pe_acc=True)
                            S.op("pe", lambda e, hh=hh, d=d: e.matmul(psP[hh].ap[:, qs:512], ntri.ap[:], d["sp"].ap[:, qs:512], start=first, stop=True, skip_group_check=True),
                                 reads=[ntri, d["sp"]], writes=[psP[hh]], pe_acc=(not first))
                            prev_sp[hh] = (d["sp"], qs)
                        for hh in range(2):
                            d = s_["hs"][hh]
                            d["xb"] = Xb.next()
                            S.op("act", lambda e, hh=hh, d=d: e.activation(out=d["xb"].ap[:, qs:512], in_=psP[hh].ap[:, qs:512], func=AF.Exp), reads=[psP[hh]], writes=[d["xb"]])
                        for hh in range(2):
                            d = s_["hs"][hh]
                            d["ab"] = Ab.next()
                            S.op("dve", lambda e, d=d: e.tensor_tensor(out=d["ab"].ap[:, qs:512], in0=d["eb"].ap[:, qs:512], in1=d["xb"].ap[:, qs:512], op=ALU.mult),
                                 reads=[d["eb"], d["xb"]], writes=[d["ab"]])
                        if o >= 0:
                            for hh in range(2):
                                d = s_["hs"][hh]
                                S.op("pool", lambda e, d=d: e.tensor_tensor(out=d["ab"].ap[:, qs:qs + 128], in0=d["ab"].ap[:, qs:qs + 128], in1=trimask.ap[:], op=ALU.mult),
                                     reads=[d["ab"], trimask], writes=[d["ab"]])

                    def stage3(u):
                        s_ = state.pop(u)
                        G, j, qs = s_["G"], s_["j"], s_["qs"]
                        first = (j == 4 * G + 3)
                        for hh in range(2):
                            d = s_["hs"][hh]
                            S.op("pe", lambda e, hh=hh, d=d: e.matmul(psY.ap[hh * 64:(hh + 1) * 64, qs:512], v_.ap[:, j, hh * 64:(hh + 1) * 64], d["ab"].ap[:, qs:512],
                                                                      start=first, stop=(j == 0), skip_group_check=True),
                                 reads=[v_, d["ab"]], writes=[psY], pe_acc=(not first))
                        if j == 0:
                            S.op("dve", lambda e: e.tensor_copy(out=y_.ap[:, G * 512:(G + 1) * 512], in_=psY.ap[:]), reads=[psY], writes=[y_])

                    U = len(steps)
                    for s in range(U + 4):
                        if s < U:
                            stage1(s)
                        if 0 <= s - 2 < U:
                            stage2(s - 2)
                        if 0 <= s - 4 < U:
                            stage3(s - 4)
                        next(gate_gen, None)
                        if s % 6 == 3:
                            next(cgen, None)
                    S.dma("sp", lambda e, y_=y_, hp=hp: e.dma_start(out=YTd[4 + hp, :, :], in_=y_.ap[:]), reads=[y_])
                for _ in gate_gen:
                    pass
                for _ in cgen:
                    pass
                S.barrier(dummy)

        def phase_C(l):
            with contextlib.ExitStack() as ph:
                wa = Buf(sb(ph, "D_wa", [128, 4, D], BF16))
                wbb = Buf(sb(ph, "D_wb", [128, 4, D], BF16))
                wo = Buf(sb(ph, "D_wo", [128, 8, D], BF16))
                Yin = Rot([Buf(sb(ph, "D_y%d" % i, [128, 8, GT], BF16)) for i in range(1)])
                Gin = Rot([Buf(sb(ph, "D_g%d" % i, [128, 16, GT], BF16)) for i in range(1)])
                bufA = Buf(sb(ph, "D_bA", [128, 8, GT], F32))
                bufB = Buf(sb(ph, "D_bB", [128, 8, GT], F32))
                bufC = Buf(sb(ph, "D_bC", [128, 8, GT], F32))
                mrg = Buf(sb(ph, "D_m", [128, 8, GT], BF16))
                t1 = Rot([Buf(sb(ph, "D_t1%d" % i, [128, GT], BF16)) for i in range(2)])
                t2 = Rot([Buf(sb(ph, "D_t2%d" % i, [128, GT], BF16)) for i in range(2)])
                tmp512 = Buf(sb(ph, "D_tmp", [128, GT], F32))
                rstd = Buf(sb(ph, "D_rstd", [128, GT], F32))
                psa = Rot([Buf(ps(ph, "D_pa%d" % i, [128, 512])) for i in range(3)])
                psb = psa
                psR = Buf(ps(ph, "D_pR", [128, 512]))
                pso = Rot([Buf(ps(ph, "D_po%d" % i, [128, 512])) for i in range(1)])
                psTk = Buf(ps(ph, "D_pT", [128, 1024], BF16))
                tkst = Rot([Buf(sb(ph, "D_tk%d" % i, [128, D], BF16)) for i in range(2)])
                psA = Buf(ps(ph, "D_pA", [128, 512]))
                psB = Buf(ps(ph, "D_pB", [128, 512]))
                S.dma("sp", lambda e: e.dma_start(out=wa.ap[:], in_=wb_upa[l].rearrange("(c p) n -> p c n", p=128)), writes=[wa])
                S.dma("sp", lambda e: e.dma_start(out=wbb.ap[:], in_=wb_upb[l].rearrange("(c p) n -> p c n", p=128)), writes=[wbb])
                S.dma("sp", lambda e: e.dma_start(out=wo.ap[:], in_=wb_out[l].rearrange("(c p) n -> p c n", p=128)), writes=[wo])
                xsrc = (lambda g: xT_in[:, g * GT:(g + 1) * GT].rearrange("(c p) n -> p c n", p=128)) if l == 0 else \
                       (lambda g: xres[:, :, g * GT:(g + 1) * GT].rearrange("c p n -> p c n"))

                def loads(g):
                    y_ = Yin.next()
                    S.dma("sp", lambda e: e.dma_start(out=y_.ap[:], in_=YTd[:, :, g * GT:(g + 1) * GT].rearrange("c p n -> p c n")), writes=[y_])
                    return y_

                pre = {}

                def preload(g):
                    y_ = loads(g)
                    g_ = Gin.next()
                    S.dma("sp", lambda e, g=g, g_=g_: e.dma_start(out=g_.ap[:], in_=gTd[:, :, g * GT:(g + 1) * GT].rearrange("c p n -> p c n")), writes=[g_])
                    S.op("act", lambda e, g_=g_: e.activation(out=g_.ap[:], in_=g_.ap[:], func=AF.Sigmoid), reads=[g_], writes=[g_])
                    pre[g] = (y_, g_)

                def G1(g):
                    y_, g_ = pre.pop(g)
                    for dc in range(8):
                        pa, pb = psa.next(), psb.next()
                        for kc in range(4):
                            S.op("pe", lambda e, pa=pa, kc=kc, dc=dc: e.matmul(pa.ap[:], wa.ap[:, kc, dc * 128:(dc + 1) * 128], y_.ap[:, kc, :], start=(kc == 0), stop=(kc == 3)),
                                 reads=[wa, y_], writes=[pa], pe_acc=(kc > 0))
                        for kc in range(4):
                            S.op("pe", lambda e, pb=pb, kc=kc, dc=dc: e.matmul(pb.ap[:], wbb.ap[:, kc, dc * 128:(dc + 1) * 128], y_.ap[:, 4 + kc, :], start=(kc == 0), stop=(kc == 3)),
                                 reads=[wbb, y_], writes=[pb], pe_acc=(kc > 0))
                        a1, a2 = t1.next(), t2.next()
                        S.op("dve", lambda e, a1=a1, pa=pa, dc=dc: e.tensor_tensor(out=a1.ap[:], in0=g_.ap[:, dc, :], in1=pa.ap[:], op=ALU.mult), reads=[g_, pa], writes=[a1])
                        S.op("dve", lambda e, a2=a2, pb=pb, dc=dc: e.tensor_tensor(out=a2.ap[:], in0=g_.ap[:, 8 + dc, :], in1=pb.ap[:], op=ALU.mult), reads=[g_, pb], writes=[a2])
                        S.op("dve", lambda e, a1=a1, a2=a2, dc=dc: e.tensor_tensor(out=mrg.ap[:, dc, :], in0=a1.ap[:], in1=a2.ap[:], op=ALU.add), reads=[a1, a2], writes=[mrg])
                    if g + 1 < NG:
                        preload(g + 1)

                def xload(g):
                    S.dma("sp", lambda e, g=g: e.dma_start(out=bufA.ap[:], in_=xsrc(g)), writes=[bufA])

                def G2(g):
                    for dc in range(8):
                        po = pso.next()
                        for kc in range(8):
                            S.op("pe", lambda e, po=po, kc=kc, dc=dc: e.matmul(po.ap[:], wo.ap[:, kc, dc * 128:(dc + 1) * 128], mrg.ap[:, kc, :], start=(kc == 0), stop=(kc == 7)),
                                 reads=[wo, mrg], writes=[po], pe_acc=(kc > 0))
                        S.op("dve", lambda e, po=po, dc=dc: e.scalar_tensor_tensor(out=bufB.ap[:, dc, :], in0=bufA.ap[:, dc, :], scalar=ALPHA, in1=po.ap[:], op0=ALU.mult, op1=ALU.add),
                             reads=[bufA, po], writes=[bufB])
                    if g + 1 < NG:
                        xload(g + 1)

                def TK(g):
                    for sub in range(4):
                        for c in range(8):
                            S.op("pe", lambda e, sub=sub, c=c: e.transpose(psTk.ap[:, c * 128:(c + 1) * 128], xbf[g].ap[:, c, sub * 128:(sub + 1) * 128], identb.ap[:]),
                                 reads=[xbf[g], identb], writes=[psTk])
                        tk = tkst.next()
                        if sub % 2 == 0:
                            S.op("act", lambda e, tk=tk: e.activation(out=tk.ap[:], in_=psTk.ap[:], func=AF.Copy), reads=[psTk], writes=[tk])
                        else:
                            S.op("dve", lambda e, tk=tk: e.tensor_copy(out=tk.ap[:], in_=psTk.ap[:]), reads=[psTk], writes=[tk])
                        S.dma("sp", lambda e, tk=tk, sub=sub, g=g: e.dma_start(out=x1tok[g * GT + sub * 128:g * GT + (sub + 1) * 128, :], in_=tk.ap[:]), reads=[tk])

                def LN(g, part):
                    layer_norm(ph, l, 0, g, bufB, bufB, bufC, psA, psB, tmp512, rstd, final=False, part=part)

                def RT(g):
                    for ii in range(4):
                        i = g * 4 + ii
                        for c in range(8):
                            S.op("pe", lambda e, ii=ii, c=c, i=i: e.matmul(psR.ap[:, i * 16:(i + 1) * 16], bufC.ap[:, c, ii * 128:(ii + 1) * 128], wr32.ap[:, c, :], start=(c == 0), stop=(c == 7)),
                                 reads=[bufC, wr32], writes=[psR], pe_acc=(c > 0))

                preload(0)
                xload(0)
                G1(0)
                G2(0)
                LN(0, 1)
                for g in range(1, NG):
                    LN(g - 1, 2)
                    G1(g)
                    G2(g)
                    RT(g - 1)
                    LN(g, 1)
                    TK(g - 1)
                LN(NG - 1, 2)
                RT(NG - 1)
                TK(NG - 1)
                S.op("dve", lambda e: e.tensor_tensor(out=lg_glob.ap[:], in0=psR.ap[:].rearrange("p (t e) -> p t e", e=NE), in1=brb_sb.ap[:].unsqueeze(1).to_broadcast([128, 32, NE]), op=ALU.add),
                     reads=[psR, brb_sb], writes=[lg_glob])
                S.barrier(dummy)

        def phase_D(l, final):
            with contextlib.ExitStack() as pz:
                NT = 32
                sel_all = Buf(sb(pz, "R_sel", [128, NT, NE], F32))
                m1_all = Buf(sb(pz, "R_m1", [128, NT, NE], F32))
                comb_all = Buf(sb(pz, "R_cmb", [128, NT, NE], F32))
                rank_all = Buf(sb(pz, "R_rank", [128, NT, NE], F32))
                tmp_all = Buf(sb(pz, "R_tmp", [128, NT, NE], F32))
                posw = Buf(sb(pz, "R_posw", [128, 4, NT], F32))
                idxAB = Buf(sb(pz, "R_idx", [128, 2, NT], I32))
                rowsAB = Buf(sb(pz, "R_rows", [128, 2, NT, 2], F32))
                widx_f = Buf(sb(pz, "R_wf", [128, NTL], F32))
                widx = Buf(sb(pz, "R_wi", [128, NTL], I32))
                tmpT = Buf(sb(pz, "R_tmpT", [128, NTL, NE], F32))
                sm16 = Buf(sb(pz, "R_sm", [128, 8 * NE], F32))
                with contextlib.ExitStack() as ph:
                    ex_all = Buf(sb(ph, "R_ex", [128, NT, NE], F32))
                    em_all = Buf(sb(ph, "R_em", [128, NT, NE], F32))
                    selb_all = Buf(sb(ph, "R_selb", [128, NT, NE], BF16))
                    cum_a = Buf(sb(ph, "R_cuma", [128, NT, NE], F32))
                    cum_b = Buf(sb(ph, "R_cumb", [128, NT, NE], F32))
                    cumb_bf = Buf(sb(ph, "R_cumbf", [128, NT, NE], BF16))
                    ssum = Buf(sb(ph, "R_ssum", [128, NE], F32))
                    ssum_bf = Buf(sb(ph, "R_ssumb", [128, NE], BF16))
                    g4 = Buf(sb(ph, "R_g4", [128, NT, 4], F32))
                    v32 = Buf(sb(ph, "R_v32", [128, 6, NT], F32))
                    psK = Rot([Buf(ps(ph, "R_pk%d" % i, [128, 512])) for i in range(2)])
                    slot_z = Buf(slotmap, "slot_z")
                    S.dma("sp", lambda e: e.dma_start(out=slotmap.rearrange("(p a) b -> p (a b)", p=128), in_=zeros.ap[:, 0:NSLOT * 2 // 128]), reads=[zeros], writes=[slot_z])
                    mx, gm, top1, top2, den = (v32.ap[:, k, :] for k in range(5))
                    bc16 = lambda ap: ap.unsqueeze(2).to_broadcast([128, NT, NE])
                    v3 = lambda buf: buf.ap[:]
                    g44 = lambda buf: buf.ap[:].rearrange("p t (a b) -> p (t a) b", b=4)
                    S.op("dve", lambda e: e.tensor_reduce(out=mx, in_=lg_glob.ap[:], axis=AX.X, op=ALU.max), reads=[lg_glob], writes=[v32])
                    S.op("dve", lambda e: e.tensor_tensor(out=ex_all.ap[:], in0=lg_glob.ap[:], in1=bc16(mx), op=ALU.subtract), reads=[lg_glob, v32], writes=[ex_all])
                    S.op("act", lambda e: e.activation(out=ex_all.ap[:], in_=ex_all.ap[:], func=AF.Exp), reads=[ex_all], writes=[ex_all])
                    S.op("dve", lambda e: e.tensor_reduce(out=g4.ap[:].rearrange("p t a -> p (t a)"), in_=g44(ex_all), axis=AX.X, op=ALU.max), reads=[ex_all], writes=[g4])
                    S.op("dve", lambda e: e.tensor_reduce(out=gm, in_=g4.ap[:], axis=AX.X, op=ALU.max), reads=[g4], writes=[v32])
                    S.op("dve", lambda e: e.tensor_tensor(out=g4.ap[:], in0=g4.ap[:], in1=gm.unsqueeze(2).to_broadcast([128, NT, 4]), op=ALU.is_ge), reads=[g4, v32], writes=[g4])
                    S.op("dve", lambda e: e.tensor_tensor(out=g44(em_all), in0=g44(ex_all), in1=g4.ap[:].rearrange("p t a -> p (t a)").unsqueeze(2).to_broadcast([128, NT * 4, 4]), op=ALU.mult),
                         reads=[ex_all, g4], writes=[em_all])
                    S.op("dve", lambda e: e.tensor_reduce(out=top1, in_=em_all.ap[:], axis=AX.X, op=ALU.max), reads=[em_all], writes=[v32])
                    S.op("dve", lambda e: e.tensor_tensor(out=m1_all.ap[:], in0=em_all.ap[:], in1=bc16(top1), op=ALU.is_ge), reads=[em_all, v32], writes=[m1_all])
                    S.op("dve", lambda e: e.tensor_tensor(out=tmp_all.ap[:], in0=em_all.ap[:], in1=m1_all.ap[:], op=ALU.mult), reads=[em_all, m1_all], writes=[tmp_all])
                    S.op("dve", lambda e: e.tensor_tensor(out=em_all.ap[:], in0=em_all.ap[:], in1=tmp_all.ap[:], op=ALU.subtract), reads=[em_all, tmp_all], writes=[em_all])
                    S.op("dve", lambda e: e.tensor_reduce(out=top2, in_=em_all.ap[:], axis=AX.X, op=ALU.max), reads=[em_all], writes=[v32])
                    S.op("dve", lambda e: e.tensor_tensor(out=sel_all.ap[:], in0=em_all.ap[:], in1=bc16(top2), op=ALU.is_ge), reads=[em_all, v32], writes=[sel_all])
                    S.op("dve", lambda e: e.tensor_tensor(out=comb_all.ap[:], in0=sel_all.ap[:], in1=m1_all.ap[:], op=ALU.add), reads=[sel_all, m1_all], writes=[comb_all])
                    S.op("dve", lambda e: e.tensor_tensor(out=den, in0=top1, in1=top2, op=ALU.add), reads=[v32], writes=[v32])
                    S.op("dve", lambda e: e.reciprocal(out=den, in_=den), reads=[v32], writes=[v32])
                    S.op("dve", lambda e: e.tensor_tensor(out=posw.ap[:, 2, :], in0=top1, in1=den, op=ALU.mult), reads=[v32], writes=[posw])
                    S.op("dve", lambda e: e.tensor_tensor(out=posw.ap[:, 3, :], in0=top2, in1=den, op=ALU.mult), reads=[v32], writes=[posw])
                    S.op("act", lambda e: e.activation(out=selb_all.ap[:], in_=comb_all.ap[:], func=AF.Copy), reads=[comb_all], writes=[selb_all])
                    S.op("dve", lambda e: e.memset(cum_a.ap[:, 0:1, :], 0.0), writes=[cum_a])
                    S.op("dve", lambda e: e.tensor_copy(out=cum_a.ap[:, 1:NT, :], in_=comb_all.ap[:, 0:NT - 1, :]), reads=[comb_all], writes=[cum_a])
                    ca, cb_ = cum_a, cum_b
                    for sh in (1, 2, 4, 8, 16):
                        S.op("dve", lambda e, ca=ca, cb_=cb_, sh=sh: e.tensor_copy(out=cb_.ap[:, 0:sh, :], in_=ca.ap[:, 0:sh, :]), reads=[ca], writes=[cb_])
                        S.op("dve", lambda e, ca=ca, cb_=cb_, sh=sh: e.tensor_tensor(out=cb_.ap[:, sh:NT, :], in0=ca.ap[:, sh:NT, :], in1=ca.ap[:, 0:NT - sh, :], op=ALU.add), reads=[ca], writes=[cb_])
                        ca, cb_ = cb_, ca
                    S.op("act", lambda e, ca=ca: e.activation(out=cumb_bf.ap[:], in_=ca.ap[:], func=AF.Copy), reads=[ca], writes=[cumb_bf])
                    S.op("dve", lambda e, ca=ca: e.tensor_tensor(out=ssum.ap[:], in0=ca.ap[:, NT - 1, :], in1=comb_all.ap[:, NT - 1, :], op=ALU.add), reads=[ca, comb_all], writes=[ssum])
                    S.op("dve", lambda e: e.tensor_copy(out=ssum_bf.ap[:], in_=ssum.ap[:]), reads=[ssum], writes=[ssum_bf])
                    pk = psK.next()
                    S.op("pe", lambda e, pk=pk: e.matmul(pk.ap[:], pstr.ap[:], selb_all.ap[:].rearrange("p t e -> p (t e)"), start=True, stop=False), reads=[pstr, selb_all], writes=[pk])
                    S.op("pe", lambda e, pk=pk: e.matmul(pk.ap[:], pones.ap[:], cumb_bf.ap[:].rearrange("p t e -> p (t e)"), start=False, stop=True), reads=[pones, cumb_bf], writes=[pk], pe_acc=True)
                    S.op("act", lambda e, pk=pk: e.activation(out=rank_all.ap[:], in_=pk.ap[:].rearrange("p (t e) -> p t e", e=NE), func=AF.Copy), reads=[pk], writes=[rank_all])
                    cfin = ssum_bf
                    pk = psK.next()
                    S.op("pe", lambda e: e.matmul(pk.ap[:, 0:16], pones.ap[:], cfin.ap[:], start=True, stop=True), reads=[pones, cfin], writes=[pk])
                    n_ = sm16.ap[:, 0:16]
                    T_ = sm16.ap[:, 16:32]
                    sc = [sm16.ap[:, 32:48], sm16.ap[:, 48:64]]
                    base_ = sm16.ap[:, 64:80]
                    S.op("dve", lambda e: e.tensor_copy(out=n_, in_=pk.ap[:, 0:16]), reads=[pk], writes=[sm16])
                    S.op("dve", lambda e: e.tensor_scalar(out=T_, in0=n_, scalar1=0.0, scalar2=None, op0=ALU.is_gt), reads=[sm16], writes=[sm16])
                    for m in range(1, S_LEN // TS):
                        S.op("dve", lambda e, m=m: e.scalar_tensor_tensor(out=T_, in0=n_, scalar=float(TS) * m, in1=T_, op0=ALU.is_gt, op1=ALU.add), reads=[sm16], writes=[sm16])
                    S.op("dve", lambda e: e.tensor_copy(out=sc[0], in_=T_), reads=[sm16], writes=[sm16])
                    cur = 0
                    for sh in (1, 2, 4, 8):
                        a, b_ = sc[cur], sc[1 - cur]
                        S.op("dve", lambda e, a=a, b_=b_, sh=sh: e.tensor_copy(out=b_[:, 0:sh], in_=a[:, 0:sh]), reads=[sm16], writes=[sm16])
                        S.op("dve", lambda e, a=a, b_=b_, sh=sh: e.tensor_tensor(out=b_[:, sh:16], in0=a[:, sh:16], in1=a[:, 0:16 - sh], op=ALU.add), reads=[sm16], writes=[sm16])
                        cur = 1 - cur
                    cumT = sc[cur]
                    S.op("dve", lambda e: e.tensor_tensor(out=base_, in0=cumT, in1=T_, op=ALU.subtract), reads=[sm16], writes=[sm16])
                    S.op("dve", lambda e: e.tensor_scalar(out=base_, in0=base_, scalar1=float(TS), scalar2=None, op0=ALU.mult), reads=[sm16], writes=[sm16])
                    S.op("dve", lambda e: e.tensor_tensor(out=rank_all.ap[:], in0=rank_all.ap[:], in1=base_.unsqueeze(1).to_broadcast([128, NT, NE]), op=ALU.add),
                         reads=[rank_all, sm16], writes=[rank_all])
                    for k, (src, msk) in enumerate(((rank_all, m1_all), (rank_all, sel_all))):
                        S.op("dve", lambda e, src=src, msk=msk: e.tensor_tensor(out=tmp_all.ap[:], in0=src.ap[:], in1=msk.ap[:], op=ALU.mult), reads=[src, msk], writes=[tmp_all])
                        S.op("dve", lambda e, k=k: e.tensor_reduce(out=posw.ap[:, k, :], in_=tmp_all.ap[:], axis=AX.X, op=ALU.add), reads=[tmp_all], writes=[posw])
                    S.op("dve", lambda e: e.tensor_copy(out=idxAB.ap[:], in_=posw.ap[:, 0:2, :]), reads=[posw], writes=[idxAB])
                    for k in range(2):
                        S.op("pool", lambda e, k=k: e.tensor_copy(out=rowsAB.ap[:, k, :, 0], in_=tokid.ap[:]), reads=[tokid], writes=[rowsAB])
                        S.op("pool", lambda e, k=k: e.tensor_copy(out=rowsAB.ap[:, k, :, 1], in_=posw.ap[:, 2 + k, :]), reads=[posw], writes=[rowsAB])
                    S.op("dve", lambda e: e.tensor_tensor(out=tmpT.ap[:], in0=tvals.ap[:], in1=cumT.unsqueeze(1).to_broadcast([128, NTL, NE]), op=ALU.is_ge),
                         reads=[tvals, sm16], writes=[tmpT])
                    S.op("dve", lambda e: e.tensor_reduce(out=widx_f.ap[:], in_=tmpT.ap[:], axis=AX.X, op=ALU.add), reads=[tmpT], writes=[widx_f])
                    S.op("dve", lambda e: e.tensor_scalar(out=widx_f.ap[:], in0=widx_f.ap[:], scalar1=15.0, scalar2=128.0, op0=ALU.min, op1=ALU.mult), reads=[widx_f], writes=[widx_f])
                    S.op("dve", lambda e: e.tensor_scalar(out=widx_f.ap[:], in0=widx_f.ap[:], scalar1=pidx.ap[:, 0:1], scalar2=None, op0=ALU.add), reads=[widx_f, pidx], writes=[widx_f])
                    S.op("dve", lambda e: e.tensor_copy(out=widx.ap[:], in_=widx_f.ap[:]), reads=[widx_f], writes=[widx])
                    for k in range(2):
                        for i in range(NT):
                            S.dma("pool", lambda e, k=k, i=i: e.indirect_dma_start(out=slotmap[:, :], out_offset=bass.IndirectOffsetOnAxis(ap=idxAB.ap[:, k, i:i + 1], axis=0),
                                                                                   in_=rowsAB.ap[:, k, i, :], in_offset=None),
                                  reads=[idxAB, rowsAB, slot_z])
                    S.barrier(dummy)
                with contextlib.ExitStack() as ph:
                    wg = Rot([Buf(sb(ph, "E_wg%d" % i, [128, 8, DF], BF16)) for i in range(2)])
                    wu = Rot([Buf(sb(ph, "E_wu%d" % i, [128, 8, DF], BF16)) for i in range(2)])
                    wd = Rot([Buf(sb(ph, "E_wd%d" % i, [128, 4, D], BF16)) for i in range(2)])
                    small = Buf(sb(ph, "E_sm", [128, NTL, SUB, 2], F32))
                    tkall = Buf(sb(ph, "E_ti", [128, NTL, SUB], I32))
                    xg_t = [sb(ph, "E_xg%d" % i, [128, SUB, D], BF16) for i in range(3)]
                    xg = Rot([[Buf(t_[:, s_, :]) for s_ in range(SUB)] for t_ in xg_t])
                    sm_ld = []
                    for t0_ in range(0, NTL, 8):
                        S.dma("sp", lambda e, t0_=t0_: e.dma_start(out=small.ap[:, t0_:t0_ + 8, :, :],
                                                                   in_=slotmap[t0_ * TS:(t0_ + 8) * TS, :].rearrange("(t s p) b -> p t s b", p=128, s=SUB)), writes=[small])
                    S.op("dve", lambda e: e.tensor_copy(out=tkall.ap[:], in_=small.ap[:, :, :, 0]), reads=[small], writes=[tkall])
                    xgT = Rot([Buf(sb(ph, "E_xT%d" % i, [128, 8, TS], BF16)) for i in range(2)])
                    hb = Rot([Buf(sb(ph, "E_h%d" % i, [128, 4, TS], BF16)) for i in range(2)])
                    s1b = Rot([Buf(sb(ph, "E_s%d" % i, [128, TS], BF16)) for i in range(3)])
                    ost = Rot([Buf(sb(ph, "E_o%d" % i, [128, SUB, D], F32)) for i in range(3)])
                    psT = Rot([Buf(ps(ph, "E_pT%d" % i, [128, 512])) for i in range(2)])
                    psg = Rot([Buf(ps(ph, "E_pg%d" % i, [128, 512])) for i in range(2)])
                    psu = Rot([Buf(ps(ph, "E_pu%d" % i, [128, 512])) for i in range(2)])
                    pso = Rot([Buf(ps(ph, "E_po%d" % i, [128, 512])) for i in range(2)])

                    def pre_w(t):
                        a, b_, c_ = wg.next(), wu.next(), wd.next()
                        for dst, srcw in ((a, wb_g[l]), (b_, wb_u[l]), (c_, wb_d[l])):
                            S.dma("pool", lambda e, dst=dst, srcw=srcw, t=t: e.indirect_dma_start(out=dst.ap[:].rearrange("p c n -> p (c n)"), out_offset=None, in_=srcw[:, :],
                                                                                                in_offset=bass.IndirectOffsetOnAxis(ap=widx.ap[:, t:t + 1], axis=0)),
                                  reads=[widx], writes=[dst])
                        return a, b_, c_

                    def pre_x(t):
                        x_ = xg.next()
                        for s_ in range(SUB):
                            S.dma("pool", lambda e, x_=x_, t=t, s_=s_: e.indirect_dma_start(out=x_[s_].ap, out_offset=None, in_=x1tok[:, :],
                                                                                            in_offset=bass.IndirectOffsetOnAxis(ap=tkall.ap[:, t, s_:s_ + 1], axis=0)),
                                  reads=[tkall], writes=[x_[s_]])
                        return x_

                    kev = [0]
                    KP = 512 // TS

                    def TT(t, x_):
                        xT = xgT.next()
                        for k0 in range(0, 8, KP):
                            pT = psT.next()
                            for kk in range(KP):
                                kc = k0 + kk
                                for s_ in range(SUB):
                                    S.op("pe", lambda e, pT=pT, kc=kc, kk=kk, s_=s_: e.matmul(pT.ap[:, kk * TS + s_ * 128:kk * TS + (s_ + 1) * 128], x_[s_].ap[:, kc * 128:(kc + 1) * 128], identb.ap[:], start=True, stop=True),
                                         reads=[x_[s_], identb], writes=[pT])
                            kev[0] += 1
                            if kev[0] % 2 == 0:
                                S.op("act", lambda e, pT=pT, k0=k0: e.activation(out=xT.ap[:, k0:k0 + KP, :], in_=pT.ap[:].rearrange("p (k n) -> p k n", k=KP), func=AF.Copy), reads=[pT], writes=[xT])
                            else:
                                S.op("dve", lambda e, pT=pT, k0=k0: e.tensor_copy(out=xT.ap[:, k0:k0 + KP, :], in_=pT.ap[:].rearrange("p (k n) -> p k n", k=KP)), reads=[pT], writes=[xT])
                        return xT

                    def GU(t, a, b_, xT):
                        h = hb.next()
                        for fc in range(4):
                            pg, pu = psg.next(), psu.next()
                            for kc in range(8):
                                S.op("pe", lambda e, pg=pg, kc=kc, fc=fc: e.matmul(pg.ap[:, 0:TS], a.ap[:, kc, fc * 128:(fc + 1) * 128], xT.ap[:, kc, :], start=(kc == 0), stop=(kc == 7)),
                                     reads=[a, xT], writes=[pg], pe_acc=(kc > 0))
                            for kc in range(8):
                                S.op("pe", lambda e, pu=pu, kc=kc, fc=fc: e.matmul(pu.ap[:, 0:TS], b_.ap[:, kc, fc * 128:(fc + 1) * 128], xT.ap[:, kc, :], start=(kc == 0), stop=(kc == 7)),
                                     reads=[b_, xT], writes=[pu], pe_acc=(kc > 0))
                            s1 = s1b.next()
                            S.op("act", lambda e, pg=pg, s1=s1: e.activation(out=s1.ap[:], in_=pg.ap[:, 0:TS], func=AF.Silu), reads=[pg], writes=[s1])
                            S.op("dve", lambda e, s1=s1, pu=pu, fc=fc: e.tensor_tensor(out=h.ap[:, fc, :], in0=s1.ap[:], in1=pu.ap[:, 0:TS], op=ALU.mult), reads=[s1, pu], writes=[h])
                        return h

                    def DN(t, c_, h):
                        o_ = ost.next()
                        for s_ in range(SUB):
                            for hf in range(2):
                                po = pso.next()
                                for fc in range(4):
                                    S.op("pe", lambda e, po=po, fc=fc, s_=s_, hf=hf: e.matmul(po.ap[:], h.ap[:, fc, s_ * 128:(s_ + 1) * 128], c_.ap[:, fc, hf * 512:(hf + 1) * 512], start=(fc == 0), stop=(fc == 3)),
                                         reads=[h, c_], writes=[po], pe_acc=(fc > 0))
                                kev[0] += 1
                                if kev[0] % 2 == 0:
                                    S.op("act", lambda e, po=po, s_=s_, hf=hf: e.activation(out=o_.ap[:, s_, hf * 512:(hf + 1) * 512], in_=po.ap[:], func=AF.Copy, scale=small.ap[:, t, s_, 1:2]),
                                         reads=[po, small], writes=[o_])
                                else:
                                    S.op("dve", lambda e, po=po, s_=s_, hf=hf: e.tensor_scalar(out=o_.ap[:, s_, hf * 512:(hf + 1) * 512], in0=po.ap[:], scalar1=small.ap[:, t, s_, 1:2], scalar2=None, op0=ALU.mult),
                                         reads=[po, small], writes=[o_])
                        S.dma("sp", lambda e, o_=o_, t=t: e.dma_start(out=oslot[t * TS:(t + 1) * TS, :].rearrange("(s p) n -> p s n", p=128), in_=o_.ap[:]), reads=[o_])

                    xq = {0: pre_x(0), 1: pre_x(1)}
                    wq = {0: pre_w(0)}
                    xTq = {0: TT(0, xq.pop(0))}
                    for t in range(NTL):
                        a, b_, c_ = wq.pop(t)
                        if t + 1 < NTL:
                            wq[t + 1] = pre_w(t + 1)
                        if t + 2 < NTL:
                            xq[t + 2] = pre_x(t + 2)
                        h = GU(t, a, b_, xTq.pop(t))
                        if t + 1 < NTL:
                            xTq[t + 1] = TT(t + 1, xq.pop(t + 1))
                        DN(t, c_, h)
                    S.barrier(dummy)
                with contextlib.ExitStack() as ph:
                    bufA = Buf(sb(ph, "F_bA", [128, 8, GT], F32))
                    bufB = Buf(sb(ph, "F_bB", [128, 8, GT], F32))
                    bufC = Buf(sb(ph, "F_bC", [128, 8, GT], F32))
                    x32 = Buf(sb(ph, "F_x32", [128, 8, GT], F32))
                    o1 = Rot([Buf(sb(ph, "F_o1%d" % i, [128, D], F32)) for i in range(8)])
                    o2 = Rot([Buf(sb(ph, "F_o2%d" % i, [128, D], F32)) for i in range(2)])
                    tmp512 = Buf(sb(ph, "F_tmp", [128, GT], F32))
                    rstd = Buf(sb(ph, "F_rstd", [128, GT], F32))
                    psX = Rot([Buf(ps(ph, "F_px%d" % i, [128, 512])) for i in range(4)])
                    psA = Buf(ps(ph, "F_pA", [128, 512]))
                    psB = Buf(ps(ph, "F_pB", [128, 512]))
                    gath = {}

                    def GA(g):
                        lst = []
                        for ii in range(4):
                            i = g * 4 + ii
                            a1, a2 = o1.next(), o2.next()
                            S.dma("pool", lambda e, a1=a1, i=i: e.indirect_dma_start(out=a1.ap[:, :], out_offset=None, in_=oslot[:, :],
                                                                                     in_offset=bass.IndirectOffsetOnAxis(ap=idxAB.ap[:, 0, i:i + 1], axis=0)), reads=[idxAB], writes=[a1])
                            S.dma("pool", lambda e, a2=a2, i=i: e.indirect_dma_start(out=a2.ap[:, :], out_offset=None, in_=oslot[:, :],
                                                                                     in_offset=bass.IndirectOffsetOnAxis(ap=idxAB.ap[:, 1, i:i + 1], axis=0)), reads=[idxAB], writes=[a2])
                            S.op("dve", lambda e, a1=a1, a2=a2: e.tensor_tensor(out=a1.ap[:], in0=a1.ap[:], in1=a2.ap[:], op=ALU.add), reads=[a1, a2], writes=[a1])
                            lst.append(a1)
                        gath[g] = lst

                    def xload(g):
                        S.dma("sp", lambda e, g=g: e.dma_start(out=x32.ap[:], in_=xres[:, :, g * GT:(g + 1) * GT].rearrange("c p n -> p c n")), writes=[x32])

                    def GB(g):
                        for ii in range(4):
                            a1 = gath[g][ii]
                            for hf in range(2):
                                px = psX.next()
                                for cc in range(4):
                                    c = hf * 4 + cc
                                    S.op("pe", lambda e, px=px, cc=cc, c=c, a1=a1: e.transpose(px.ap[:, cc * 128:(cc + 1) * 128], a1.ap[:, c * 128:(c + 1) * 128], ident.ap[:]),
                                         reads=[a1, ident], writes=[px])
                                S.op("dve", lambda e, px=px, hf=hf, ii=ii: e.scalar_tensor_tensor(out=bufB.ap[:, hf * 4:(hf + 1) * 4, ii * 128:(ii + 1) * 128],
                                                                                                  in0=x32.ap[:, hf * 4:(hf + 1) * 4, ii * 128:(ii + 1) * 128], scalar=ALPHA,
                                                                                                  in1=px.ap[:].rearrange("p (c n) -> p c n", n=128), op0=ALU.mult, op1=ALU.add),
                                     reads=[x32, px], writes=[bufB])
                        if g + 1 < NG:
                            xload(g + 1)

                    def LN(g, part):
                        layer_norm(ph, l, 2, g, bufB, bufA, bufC, psA, psB, tmp512, rstd, final=final, part=part)

                    xload(0)
                    GA(0)
                    GB(0)
                    LN(0, 1)
                    for g in range(1, NG):
                        GA(g)
                        LN(g - 1, 2)
                        GB(g)
                        LN(g, 1)
                    LN(NG - 1, 2)
                    S.barrier(dummy)

        S.barrier(dummy)
        for l in range(n_layers):
            with contextlib.ExitStack() as lay:
                Bm_all = [Buf(sb(lay, "Bm%d" % h, [128, 640], F32)) for h in range(8)]
                for h in range(8):
                    S.dma("sp", lambda e, h=h: e.dma_start(out=Bm_all[h].ap[:], in_=relB[l, h, :, :]), writes=[Bm_all[h]])
                    S.op("dve", lambda e, h=h: e.tensor_tensor(out=Bm_all[h].ap[:], in0=Bm_all[h].ap[:], in1=maskM.ap[:], op=ALU.add),
                         reads=[Bm_all[h], maskM], writes=[Bm_all[h]])
                phase_A(l)
                if l == 0:
                    cast_weights(0, "moe")
                if stop_after == ("A", l):
                    break
                phase_B1(l, Bm_all)
            if stop_after == ("B1", l):
                break
            phase_B2(l)
            if stop_after == ("B2", l):
                break
            phase_C(l)
            if stop_after == ("C", l):
                break
            phase_D(l, final=(l == n_layers - 1))
        if debug:
            pass
        S.finish("sp")
        S.emit()
    return nc


def _host_layout(inputs):
    f = lambda a: np.ascontiguousarray(np.asarray(a, dtype=np.float32))
    rel = f(inputs["rel_bias"])
    qi = np.arange(128)[:, None]
    cc = np.arange(640)[None, :]
    idx = np.clip(qi + 512 - cc, -128, 128) + 128
    relB = f(rel[:, :, idx])
    lnp = np.stack([f(inputs["ln1_g"]), f(inputs["ln1_b"]), f(inputs["ln2_g"]), f(inputs["ln2_b"])], axis=1)
    lnp = lnp.reshape(DEPTH, 4, 8, 128).transpose(3, 0, 1, 2).reshape(128, DEPTH * 4 * 8)
    shared = {
        "w_in": f(inputs["w_in"]), "relB": relB, "w_up_a": f(inputs["w_up_a"]), "w_up_b": f(inputs["w_up_b"]),
        "w_out": f(inputs["w_out"]), "lnp": f(lnp), "w_router": f(inputs["w_router"]),
        "brb": f(np.broadcast_to(f(inputs["b_router"])[None, :], (128, NE))),
        "w_gate": f(inputs["w_gate"]), "w_up": f(inputs["w_up"]), "w_down": f(inputs["w_down"]),
    }
    return shared


_NC_CACHE = {}


def kernel(**inputs):
    x = np.asarray(inputs["x"], dtype=np.float32)
    shared = _host_layout(inputs)
    n = x.shape[0]
    in_maps = []
    for b in range(n):
        m = dict(shared)
        m["xT"] = np.ascontiguousarray(x[b].T)
        in_maps.append(m)
    if "nc" not in _NC_CACHE:
        _NC_CACHE["nc"] = build_nc()
    res = run_bass_kernel_spmd(_NC_CACHE["nc"], in_maps, core_ids=list(range(n)))
    out = np.stack([np.ascontiguousarray(r["yT"].T) for r in res.results], axis=0)
    return out.astype(np.float32)
```
